# Optimizing a Trainium2 kernel written in Bass

```python
import math
import jax, jax.numpy as jnp
from jax import lax
import numpy as np

D_MODEL = 1024
BATCH = 4
SEQ = 4096
DEPTH = 2


CHUNK = 64
HEAD_DIM = 64
FOX_HEADS = D_MODEL // HEAD_DIM
FOX_BLOCK = 128
SWA_Q_HEADS = D_MODEL // HEAD_DIM
SWA_KV_HEADS = 4
SWA_GROUP = SWA_Q_HEADS // SWA_KV_HEADS
WINDOW = 128
WINDOW_CHUNKS = WINDOW // CHUNK
BAND = (WINDOW_CHUNKS + 1) * CHUNK
REL_BUCKETS = 32
REL_MAX_DIST = 128
D_FF = 7 * D_MODEL // 2
N_EXPERTS = 8
TOP_K = 2
N_MOD = 6
EPS = 1e-6
N_EVEN = (DEPTH + 1) // 2
N_ODD = DEPTH // 2

kernel_name = "hybrid_fox_swa_sink_moe_adaln"


def rmsnorm(x, g):
    xf = x.astype(jnp.float32)
    y = xf * lax.rsqrt(jnp.mean(xf * xf, axis=-1, keepdims=True) + EPS)
    return (y * g.astype(jnp.float32)).astype(x.dtype)


def modulate(h, shift, scale):
    return h * (1.0 + scale[:, None, :]) + shift[:, None, :]


def t5_band_buckets():
    q = np.arange(CHUNK)[:, None]
    k = np.arange(BAND)[None, :] - WINDOW_CHUNKS * CHUNK
    rel = k - q
    nb = REL_BUCKETS // 2
    max_exact = nb // 2
    ret = (rel > 0).astype(np.int32) * nb
    n = np.abs(rel)
    large = max_exact + (np.log(np.maximum(n, 1) / max_exact)
                         / np.log(REL_MAX_DIST / max_exact) * (nb - max_exact)).astype(np.int32)
    large = np.minimum(large, nb - 1)
    return (ret + np.where(n < max_exact, n, large)).astype(np.int32)


def fox_attention(xn, w_in, b_f, w_out):
    B, S, D = xn.shape
    H, Dh = FOX_HEADS, HEAD_DIM
    q, k, v, f_logit = jnp.split(xn @ w_in, [D, 2 * D, 3 * D], axis=-1)
    q = q.reshape(B, S, H, Dh) * (Dh ** -0.5)
    k = k.reshape(B, S, H, Dh)
    v = v.reshape(B, S, H, Dh)
    log_f = jax.nn.log_sigmoid(f_logit.astype(jnp.float32) + b_f.astype(jnp.float32))
    cum = jnp.cumsum(log_f, axis=1).transpose(0, 2, 1)
    outs = []
    for blk in range(S // FOX_BLOCK):
        q0, q1 = blk * FOX_BLOCK, (blk + 1) * FOX_BLOCK
        s = jnp.einsum('bqhd,bkhd->bhqk', q[:, q0:q1], k[:, :q1]).astype(jnp.float32)
        s = s + cum[:, :, q0:q1, None] - cum[:, :, None, :q1]
        causal = jnp.arange(q1)[None, :] <= jnp.arange(q0, q1)[:, None]
        s = jnp.where(causal, s, -jnp.inf)
        p = jax.nn.softmax(s, axis=-1).astype(v.dtype)
        outs.append(jnp.einsum('bhqk,bkhd->bqhd', p, v[:, :q1]))
    o = jnp.concatenate(outs, axis=1).reshape(B, S, D)
    return o @ w_out


def swa_sink_attention(xn, w_in, sinks, rel_table, w_out):
    B, S, D = xn.shape
    Hk, G, Dh = SWA_KV_HEADS, SWA_GROUP, HEAD_DIM
    nC = S // CHUNK
    kvw = Hk * Dh
    q, k, v = jnp.split(xn @ w_in, [D, D + kvw], axis=-1)
    q = q.reshape(B, nC, CHUNK, Hk, G, Dh) * (Dh ** -0.5)

    def band(t):
        t = t.reshape(B, S, Hk, Dh)
        t = jnp.pad(t, ((0, 0), (WINDOW_CHUNKS * CHUNK, 0), (0, 0), (0, 0)))
        t = t.reshape(B, nC + WINDOW_CHUNKS, CHUNK, Hk, Dh)
        return jnp.concatenate([t[:, j:j + nC] for j in range(WINDOW_CHUNKS + 1)], axis=2)

    kb, vb = band(k), band(v)
    s = jnp.einsum('bcqhgd,bckhd->bchgqk', q, kb).astype(jnp.float32)
    bias = rel_table[t5_band_buckets()]
    bias = bias.transpose(2, 0, 1).reshape(Hk, G, CHUNK, BAND).astype(jnp.float32)
    s = s + bias
    kpos = (jnp.arange(nC)[:, None] * CHUNK - WINDOW_CHUNKS * CHUNK
            + jnp.arange(BAND)[None, :])
    s = jnp.where((kpos >= 0)[None, :, None, None, None, :], s, -jnp.inf)
    sink = sinks.astype(jnp.float32).reshape(1, 1, Hk, G, 1, 1)
    m = jnp.maximum(jnp.max(s, axis=-1, keepdims=True), sink)
    p = jnp.exp(s - m)
    p = p / (jnp.sum(p, axis=-1, keepdims=True) + jnp.exp(sink - m))
    o = jnp.einsum('bchgqk,bckhd->bcqhgd', p.astype(vb.dtype), vb).reshape(B, S, D)
    return o @ w_out


def swiglu(h, w_gu, w_down):
    g, u = jnp.split(h @ w_gu, 2, axis=-1)
    return (jax.nn.silu(g) * u) @ w_down


def moe_swiglu(xn, w_router, b_router, w_gu, w_down):
    B, S, D = xn.shape
    xt = xn.reshape(B * S, D)
    logits = (xt @ w_router).astype(jnp.float32) + b_router.astype(jnp.float32)
    top_val, top_idx = lax.top_k(logits, TOP_K)
    top_w = jax.nn.softmax(top_val, axis=-1)
    gates = jnp.sum(jax.nn.one_hot(top_idx, N_EXPERTS, dtype=jnp.float32) * top_w[..., None],
                    axis=1).astype(xt.dtype)
    out = jnp.zeros_like(xt)
    for e in range(N_EXPERTS):
        out = out + gates[:, e:e + 1] * swiglu(xt, w_gu[e], w_down[e])
    return out.reshape(B, S, D)


def setup_inputs(seed: int = 0) -> dict:
    key = jax.random.key(seed)
    ks = jax.random.split(key, 20)
    D, F, E = D_MODEL, D_FF, N_EXPERTS
    kvw = SWA_KV_HEADS * HEAD_DIM
    nrm = jax.random.normal
    sd = D ** -0.5
    sf = F ** -0.5
    return {
        "x": nrm(ks[0], (BATCH, SEQ, D), jnp.float32),
        "c": nrm(ks[1], (BATCH, D), jnp.float32),
        "w_ada": nrm(ks[2], (DEPTH, D, N_MOD * D), jnp.float32) * (0.5 * sd),
        "b_ada": nrm(ks[3], (DEPTH, N_MOD * D), jnp.float32) * 0.02,
        "g_norm_mix": 1.0 + 0.05 * nrm(ks[4], (DEPTH, D), jnp.float32),
        "g_norm_ffn": 1.0 + 0.05 * nrm(ks[5], (DEPTH, D), jnp.float32),
        "g_final": 1.0 + 0.05 * nrm(ks[6], (D,), jnp.float32),
        "fox_w_in": nrm(ks[7], (N_EVEN, D, 3 * D + FOX_HEADS), jnp.float32) * sd,
        "fox_b_f": 2.0 + 0.5 * nrm(ks[8], (N_EVEN, FOX_HEADS), jnp.float32),
        "fox_w_out": nrm(ks[9], (N_EVEN, D, D), jnp.float32) * sd,
        "swa_w_in": nrm(ks[10], (N_ODD, D, D + 2 * kvw), jnp.float32) * sd,
        "swa_sinks": 0.5 * nrm(ks[11], (N_ODD, SWA_Q_HEADS), jnp.float32),
        "swa_w_out": nrm(ks[12], (N_ODD, D, D), jnp.float32) * sd,
        "rel_bias": 0.5 * nrm(ks[13], (REL_BUCKETS, SWA_Q_HEADS), jnp.float32),
        "ffn_w_gu": nrm(ks[14], (N_EVEN, D, 2 * F), jnp.float32) * sd,
        "ffn_w_down": nrm(ks[15], (N_EVEN, F, D), jnp.float32) * sf,
        "moe_w_router": nrm(ks[16], (N_ODD, D, E), jnp.float32) * sd,
        "moe_b_router": 0.01 * nrm(ks[17], (N_ODD, E), jnp.float32),
        "moe_w_gu": nrm(ks[18], (N_ODD, E, D, 2 * F), jnp.float32) * sd,
        "moe_w_down": nrm(ks[19], (N_ODD, E, F, D), jnp.float32) * sf,
    }


def reference(x, c, w_ada, b_ada, g_norm_mix, g_norm_ffn, g_final,
              fox_w_in, fox_b_f, fox_w_out,
              swa_w_in, swa_sinks, swa_w_out, rel_bias,
              ffn_w_gu, ffn_w_down,
              moe_w_router, moe_b_router, moe_w_gu, moe_w_down):
    cond = jax.nn.silu(c)
    for i in range(DEPTH):
        j = i // 2
        mod = cond @ w_ada[i] + b_ada[i]
        sh1, sc1, gt1, sh2, sc2, gt2 = jnp.split(mod, N_MOD, axis=-1)
        h = modulate(rmsnorm(x, g_norm_mix[i]), sh1, sc1)
        if i % 2 == 0:
            y = fox_attention(h, fox_w_in[j], fox_b_f[j], fox_w_out[j])
        else:
            y = swa_sink_attention(h, swa_w_in[j], swa_sinks[j], rel_bias, swa_w_out[j])
        x = x + gt1[:, None, :] * y
        h = modulate(rmsnorm(x, g_norm_ffn[i]), sh2, sc2)
        if i % 2 == 0:
            y = swiglu(h, ffn_w_gu[j], ffn_w_down[j])
        else:
            y = moe_swiglu(h, moe_w_router[j], moe_b_router[j], moe_w_gu[j], moe_w_down[j])
        x = x + gt2[:, None, :] * y
    return rmsnorm(x, g_final)
```

```python
import contextlib
import os
import numpy as np
import ml_dtypes
import concourse.bass as bass
import concourse.mybir as mybir
from concourse.bass_utils import run_bass_kernel_spmd

F32 = mybir.dt.float32
BF16 = mybir.dt.bfloat16
AF = mybir.ActivationFunctionType
ALU = mybir.AluOpType
AX = mybir.AxisListType

D = 1024
S = 4096
H = 16
DH = 64
FF = 3584
NE = 8
NOWN = 17
NCTX = 15
TOWN = NOWN * 128
TCTX = NCTX * 128
NEG = -30000.0

ENGS = ['pe', 'act', 'dve', 'pool', 'sp']
SEM_LIMIT = 30000


class Op:
    __slots__ = ('eng', 'fn', 'deps', 'isdma', 'dsem', 'count', 'needed', 'sig', 'key', 'seq')

    def __init__(self, eng, fn):
        self.eng = eng
        self.fn = fn
        self.deps = []
        self.isdma = False
        self.dsem = None
        self.count = 0
        self.needed = False
        self.sig = None
        self.key = None
        self.seq = 0


class DmaSem:
    def __init__(self, prog, name):
        self.sem = prog.new_sem(name)
        self.count = 0
        self.key = ('dma', name)
        self.hist = []

    def count_before(self, seq):
        c = 0
        for sq, cn in self.hist:
            if sq < seq:
                c = cn
            else:
                break
        return c


_DSIZE = {}


def _dsize(dt):
    if dt not in _DSIZE:
        _DSIZE[dt] = 2 if dt == BF16 else (4 if dt == F32 else int(np.dtype(dt.np).itemsize))
    return _DSIZE[dt]


def _region(ap):
    t = ap.tensor
    sp = t.space
    if sp == 'SB':
        space = 'sb'
        base = t.manual_sbuf_range[0]
    elif sp == 'PSUM':
        space = 'ps_' + t.name
        base = 0
    else:
        return None
    ds = _dsize(t.dtype)
    shape = list(t.shape)
    rowsize = 1
    for d_ in shape[1:]:
        rowsize *= d_
    apl = [(int(a), int(b)) for a, b in ap.ap]
    off = int(ap.offset)
    pcnt = apl[0][1]
    p0 = off // rowsize
    col0 = off % rowsize
    dims = sorted([(st, c) for st, c in apl[1:] if c > 1], reverse=True)
    starts = [col0]
    k = 0
    while k < len(dims) - 1 and len(starts) * dims[k][1] <= 64:
        st, c = dims[k]
        starts = [s0 + i * st for s0 in starts for i in range(c)]
        k += 1
    span = 1
    for st, c in dims[k:]:
        span += (c - 1) * st
    iv = [(base + s0 * ds, base + (s0 + span) * ds) for s0 in starts]
    lo = min(a for a, _ in iv)
    hi = max(b for _, b in iv)
    return (space, p0, p0 + pcnt, lo, hi, iv)


def _overlap(r1, r2):
    if r1[2] <= r2[1] or r2[2] <= r1[1] or r1[4] <= r2[3] or r2[4] <= r1[3]:
        return False
    i1, i2 = r1[5], r2[5]
    if len(i1) == 1 and len(i2) == 1:
        return True
    for a0, a1 in i1:
        for b0, b1 in i2:
            if a0 < b1 and b0 < a1:
                return True
    return False


def _contains(big, small):
    if big[1] > small[1] or big[2] < small[2]:
        return False
    if len(big[5]) == 1:
        return big[3] <= small[3] and big[4] >= small[4]
    if len(big[5]) == len(small[5]):
        return all(a0 <= b0 and a1 >= b1 for (a0, a1), (b0, b1) in zip(big[5], small[5]))
    return False


class Tracker:
    BK = 2048

    def __init__(self):
        self.buckets = {}
        self.rid = 0

    def _bks(self, r):
        return [(r[0], b) for b in range(r[3] // self.BK, (r[4] - 1) // self.BK + 1)]

    def access(self, op, reads, writes):
        deps = {}
        recs = []
        for ap, isw in [(a, False) for a in reads] + [(a, True) for a in writes]:
            if ap is None or isinstance(ap, (int, float)):
                continue
            r = _region(ap)
            if r is None:
                continue
            seen = set()
            for bk in self._bks(r):
                lst = self.buckets.get(bk)
                if not lst:
                    continue
                for rec in lst:
                    if rec[0] in seen:
                        continue
                    seen.add(rec[0])
                    _, rr, rop, risw = rec
                    if not (isw or risw):
                        continue
                    if rop is op:
                        continue
                    if _overlap(r, rr):
                        deps[id(rop)] = rop
            recs.append((r, isw))
        for r, isw in recs:
            self.rid += 1
            rec = (self.rid, r, op, isw)
            for bk in self._bks(r):
                lst = self.buckets.setdefault(bk, [])
                if isw:
                    lst[:] = [x for x in lst if not _contains(r, x[1])]
                else:
                    lst[:] = [x for x in lst if not ((not x[3]) and x[2].key == op.key and _contains(r, x[1]))]
                lst.append(rec)
        return list(deps.values())


class Prog:
    def __init__(self, nc):
        self.nc = nc
        self.es = contextlib.ExitStack()
        self.ops = {e: [] for e in ENGS}
        self.nsem = 0
        self.trk = Tracker()
        self.nseq = 0

    def new_sem(self, name):
        self.nsem += 1
        return self.es.enter_context(self.nc.semaphore(f"{name}_{self.nsem}"))

    def op(self, eng, fn, waits=(), signal=True, reads=(), writes=()):
        o = Op(eng, fn)
        o.key = eng
        self.nseq += 1
        o.seq = self.nseq
        deps = [w for w in waits if w is not None]
        deps += self.trk.access(o, reads, writes)
        o.deps = deps
        self.ops[eng].append(o)
        return o

    def dma(self, eng, dsem, out, in_, waits=()):
        o = Op(eng, lambda e: e.dma_start(out=out, in_=in_))
        o.isdma = True
        o.dsem = dsem
        o.key = dsem.key
        dsem.count += 16
        o.count = dsem.count
        self.nseq += 1
        o.seq = self.nseq
        dsem.hist.append((o.seq, o.count))
        deps = [w for w in waits if w is not None]
        deps += self.trk.access(o, [in_], [out])
        o.deps = deps
        self.ops[eng].append(o)
        return o

    def wait_only(self, eng, waits):
        o = Op(eng, None)
        o.key = eng
        self.nseq += 1
        o.seq = self.nseq
        o.deps = [w for w in waits if w is not None]
        self.ops[eng].append(o)

    def _reduce_deps(self, o):
        best = {}
        for d in o.deps:
            if d.isdma:
                k = d.dsem.key
                if k not in best or d.count > best[k].count:
                    best[k] = d
            else:
                if d.eng == 'pe' and o.eng == 'pe' and not o.isdma:
                    continue
                k = d.eng
                if k not in best or d.seq > best[k].seq:
                    best[k] = d
        o.deps = list(best.values())

    def emit(self):
        nc = self.nc
        for eng in ENGS:
            for o in self.ops[eng]:
                self._reduce_deps(o)
        for eng in ENGS:
            for o in self.ops[eng]:
                for d in o.deps:
                    if d.isdma:
                        continue
                    if d.eng == 'pe' and o.eng == 'pe' and not o.isdma:
                        continue
                    d.needed = True
        for eng in ENGS:
            cnt = SEM_LIMIT
            gen = 0
            sem = None
            for o in self.ops[eng]:
                if o.isdma or not o.needed:
                    continue
                if cnt >= SEM_LIMIT:
                    gen += 1
                    sem = self.new_sem(f"s_{eng}{gen}")
                    cnt = 0
                cnt += 1
                o.sig = (sem, cnt, (eng, gen))
        nwaits = [0]
        with nc.Block() as block:
            def run(eng_name):
                def body(e):
                    waited = {}
                    for o in self.ops[eng_name]:
                        for d in o.deps:
                            if d.isdma:
                                sem, count, key = d.dsem.sem, max(d.count, d.dsem.count_before(o.seq)), d.dsem.key
                            else:
                                if d.sig is None:
                                    continue
                                sem, count, key = d.sig
                            if waited.get(key, 0) >= count:
                                continue
                            waited[key] = count
                            e.wait_ge(sem, count)
                            nwaits[0] += 1
                        if o.fn is None:
                            continue
                        ins = o.fn(e)
                        if o.isdma:
                            ins.then_inc(o.dsem.sem, 16)
                        elif o.sig is not None:
                            ins.then_inc(o.sig[0], 1)
                return body
            block.tensor(run('pe'))
            block.scalar(run('act'))
            block.vector(run('dve'))
            block.gpsimd(run('pool'))
            block.sync(run('sp'))
        self.stats = {e: len(self.ops[e]) for e in ENGS}
        self.stats['waits'] = nwaits[0]

    def close(self):
        self.es.close()


class Ring:
    def __init__(self, bufs):
        self.bufs = bufs
        self.free = [[] for _ in bufs]
        self.i = 0

    def next(self):
        s = self.i % len(self.bufs)
        self.i += 1
        return s, self.bufs[s], list(self.free[s])

    def release(self, slot, evs):
        self.free[slot] = [e for e in evs if e is not None]


A0 = 0
MODB = 69632
CONST = 94208
B0 = 98304
SB_END = 212736
SB_BASE = 16544


def build_program(stage=99):
    nc = bass.Bass("TRN2", target_bir_lowering=False)
    P = Prog(nc)

    def din(name, shape, dt=F32):
        return nc.dram_tensor(name, list(shape), dt, kind="ExternalInput").ap()

    xo_d = din("xo", [TOWN, D])
    xc_d = din("xc", [TCTX, D])
    cT_d = din("cT", [128, 8])
    kb2_d = din("kb2", [2, S], BF16)
    halo_d = din("halo", [128, 1])
    ident_d = din("ident", [128, 128], BF16)
    identf_d = din("identf", [128, 128])
    trim_d = din("trimask", [128, 128], BF16)
    selk_d = din("selk", [H, 80, 71], BF16)
    selq_d = din("selq", [H, 80, 71], BF16)
    swab_d = din("swab", [128, 4 * 2 * 4 * 128], BF16)
    w_ada_d = din("w_ada", [2, D, 6 * D])
    b_ada_d = din("b_ada", [2, 6 * D])
    gmix_d = din("g_norm_mix", [2, D])
    gffn_d = din("g_norm_ffn", [2, D])
    gfin_d = din("g_final", [1, D])
    fwin_d = din("fox_w_in", [1, D, 3 * D + H])
    fbf_d = din("fox_b_f", [H, 1])
    fwout_d = din("fox_w_out", [1, D, D])
    swin_d = din("swa_w_in", [1, D, D + 512])
    ssink_d = din("swa_sinks", [1, H])
    swout_d = din("swa_w_out", [1, D, D])
    wgu_d = din("ffn_w_gu", [1, D, 2 * FF])
    wdn_d = din("ffn_w_down", [1, FF, D])
    wr_d = din("moe_w_router", [1, D, NE])
    br_d = din("moe_b_router", [1, NE])
    if stage >= 6:
        mgu_d = din("moe_w_gu", [1, NE, D, 2 * FF])
        mdn_d = din("moe_w_down", [1, NE, FF, D])
    out_d = nc.dram_tensor("out", [2048, D], F32, kind="ExternalOutput").ap()
    dbg_d = None
    if stage < 99:
        dbg_d = nc.dram_tensor("dbg", [128, NOWN * D], F32, kind="ExternalOutput").ap()
        dbg2_d = nc.dram_tensor("dbg2", [128, 8 * S], BF16, kind="ExternalOutput").ap()
        dbg3_d = nc.dram_tensor("dbg3", [128, S], F32, kind="ExternalOutput").ap()

    ntens = [0]

    def T(name, shape, dt, off):
        ntens[0] += 1
        assert off % 32 == 0, (name, off)
        sz = int(np.prod(shape[1:])) * (2 if dt == BF16 else 4)
        assert off + sz <= SB_END, (name, off, sz)
        return nc.alloc_sbuf_tensor_at(f"{name}{ntens[0]}", list(shape), dt, offset=off + SB_BASE)

    pb = [nc.alloc_psum_tensor(f"pb{i}", [128, 512], F32) for i in range(8)]
    pbb = [p.bitcast(BF16) for p in pb]

    x_res = T("x_res", [128, NOWN, D], F32, A0)
    modt = [T(f"mod{i}", [128, D], F32, MODB + i * 4096) for i in range(6)]
    SH1, GSC1, GT1, SH2, GSC2, GT2 = range(6)
    ident = T("ident", [128, 128], BF16, CONST)
    trim = T("trim", [128, 128], BF16, CONST + 256)
    cond_rep = T("cond_rep", [128, 8, 128], BF16, CONST + 512)
    condT = T("condT", [128, 8], F32, CONST + 2560)
    cond_s = T("cond_s", [128, 8], F32, CONST + 2592)
    ssq = T("ssq", [128, 64], F32, CONST + 2624)
    rstd = T("rstd", [128, 64], F32, CONST + 2880)
    halo_t = T("halo_t", [128, 1], F32, CONST + 3136)
    bf_col = T("bf_col", [H, 1], F32, CONST + 3168)
    esink = T("esink", [128, H], F32, CONST + 3200)
    brb = T("brb", [128, NE], F32, CONST + 3264)
    small = T("small", [128, 64], F32, CONST + 3296)
    identf = T("identf", [128, 128], F32, CONST + 3584)

    ds_c = DmaSem(P, "dc")
    ds_out = DmaSem(P, "dout")

    def _aps(*xs):
        return [x for x in xs if x is not None and not isinstance(x, (int, float))]

    def act(out, in_, func, waits=(), signal=True, **kw):
        return P.op('act', lambda e: e.activation(out=out, in_=in_, func=func, **kw), waits,
                    reads=_aps(in_, kw.get('bias'), kw.get('scale')), writes=_aps(out, kw.get('accum_out')))

    def mm(out, lhsT, rhs, start, stop, waits=(), signal=False):
        return P.op('pe', lambda e: e.matmul(out, lhsT=lhsT, rhs=rhs, start=start, stop=stop), waits,
                    reads=[lhsT, rhs], writes=[out])

    def tr(out, in_, idn, waits=(), signal=False):
        return P.op('pe', lambda e: e.transpose(out=out, in_=in_, identity=idn), waits, reads=[in_, idn], writes=[out])

    def vtt(out, in0, in1, op, waits=(), signal=True, eng='dve'):
        return P.op(eng, lambda e: e.tensor_tensor(out=out, in0=in0, in1=in1, op=op), waits, reads=[in0, in1], writes=[out])

    def vts(out, in0, s1, s2, op0, op1=None, waits=(), signal=True, eng='dve'):
        if op1 is None:
            return P.op(eng, lambda e: e.tensor_scalar(out=out, in0=in0, scalar1=s1, scalar2=None, op0=op0), waits,
                        reads=_aps(in0, s1), writes=[out])
        return P.op(eng, lambda e: e.tensor_scalar(out=out, in0=in0, scalar1=s1, scalar2=s2, op0=op0, op1=op1), waits,
                    reads=_aps(in0, s1, s2), writes=[out])

    def vstt(out, in0, scalar, in1, op0, op1, waits=(), signal=True, eng='dve'):
        return P.op(eng, lambda e: e.scalar_tensor_tensor(out=out, in0=in0, scalar=scalar, in1=in1, op0=op0, op1=op1), waits,
                    reads=_aps(in0, scalar, in1), writes=[out])

    def vcopy(out, in_, waits=(), signal=True, eng='dve'):
        return P.op(eng, lambda e: e.tensor_copy(out=out, in_=in_), waits, reads=[in_], writes=[out])

    def vrecip(out, in_, waits=()):
        return P.op('dve', lambda e: e.reciprocal(out=out, in_=in_), waits, reads=[in_], writes=[out])

    def memset(ap, val, waits=(), signal=True, eng='pool'):
        return P.op(eng, lambda e: e.memset(ap, val), waits, writes=[ap])

    P.dma('sp', ds_c, ident[:], ident_d)
    P.dma('sp', ds_c, identf[:], identf_d)
    P.dma('sp', ds_c, trim[:], trim_d)
    P.dma('sp', ds_c, halo_t[:], halo_d)
    P.dma('sp', ds_c, bf_col[:], fbf_d)
    P.dma('sp', ds_c, esink[:], ssink_d[0:1, :].partition_broadcast(128))
    P.dma('sp', ds_c, brb[:], br_d[0:1, :].partition_broadcast(128))
    e_const = P.dma('sp', ds_c, condT[:], cT_d)
    e_silu = act(cond_s[:], condT[:], AF.Silu, waits=[e_const])
    e_crep = None
    for k in range(8):
        e_crep = act(cond_rep[:, k, :], ident[:], AF.Identity, waits=[e_silu], scale=0.0, bias=cond_s[:, k:k + 1])
    e_esink = act(esink[:], esink[:], AF.Exp)

    def compute_mods(l, base, prior):
        wst = [T("wada", [128, 8, 512], BF16, base + i * 8192) for i in range(2)]
        bbs = [T("bb", [128, 512], F32, base + 16384 + i * 2048) for i in range(2)]
        gb = [T("gb", [128, D], F32, base + 20480 + i * 4096) for i in range(2)]
        tmp = T("modtmp", [128, 512], F32, base + 28672)
        dsw = [DmaSem(P, f"dwa{l}{i}") for i in range(2)]
        dsb = [DmaSem(P, f"dbb{l}{i}") for i in range(2)]
        dsg = DmaSem(P, f"dg{l}")
        P.dma('sp', dsg, gb[0][:], gmix_d[l:l + 1, :].partition_broadcast(128), waits=prior)
        e_g = P.dma('sp', dsg, gb[1][:], gffn_d[l:l + 1, :].partition_broadcast(128), waits=prior)
        wring = Ring(wst)
        bring = Ring(bbs)
        pring = Ring([pb[0], pb[1]])
        wv = w_ada_d[l].rearrange("(k p) n -> p k n", p=128)
        last = None
        evs = []
        for cg in range(12):
            ws, wt, wfree = wring.next()
            bs, bt, bfree = bring.next()
            ps, pt, pfree = pring.next()
            e_w = P.dma('pool', dsw[ws], wt[:], wv[:, :, cg * 512:(cg + 1) * 512], waits=wfree + list(prior))
            e_b = P.dma('sp', dsb[bs], bt[:], b_ada_d[l:l + 1, cg * 512:(cg + 1) * 512].partition_broadcast(128),
                        waits=bfree + list(prior))
            e_m = None
            for k in range(8):
                e_m = mm(pt[:], cond_rep[:, k, :], wt[:, k, :], k == 0, k == 7,
                         waits=[e_w, e_crep] + pfree, signal=(k == 7))
            idx, half = cg // 2, cg % 2
            dst = modt[idx][:, half * 512:(half + 1) * 512]
            if idx in (1, 4):
                vtt(tmp[:], pt[:], bt[:], ALU.add, waits=[e_m, e_b, last] + list(prior), signal=False)
                g = gb[0] if idx == 1 else gb[1]
                last = vstt(dst, tmp[:], 1.0, g[:, half * 512:(half + 1) * 512], ALU.add, ALU.mult, waits=[e_g])
            else:
                last = vtt(dst, pt[:], bt[:], ALU.add, waits=[e_m, e_b] + list(prior))
            wring.release(ws, [e_m])
            bring.release(bs, [last])
            pring.release(ps, [last])
            evs.append(last)
        return last

    class NormCtx:
        def __init__(self, base, prior):
            self.t = Ring([T("nt", [128, D], F32, base + i * 4096) for i in range(2)])
            self.hb = Ring([T("nhb", [128, D], BF16, base + 8192 + i * 2048) for i in range(2)])
            self.junk = T("njunk", [128, D], BF16, base + 12288)
            self.pt = Ring([pbb[6], pbb[7]])
            self.prior = list(prior)
            self.n = 0
        SIZE = 14336

    def norm_block(ctx, xin, col, gsc_t, sh_t, hT_dst, waits, h32=None):
        pr = ctx.prior if ctx.n < 2 else []
        ctx.n += 1
        e_sq = act(ctx.junk[:], xin, AF.Square, waits=list(waits) + pr, accum_out=ssq[:, col:col + 1])
        e_sd = act(rstd[:, col:col + 1], ssq[:, col:col + 1], AF.Sqrt, waits=[e_sq], scale=1.0 / D, bias=small[:, 0:1])
        e_r = vrecip(rstd[:, col:col + 1], rstd[:, col:col + 1], waits=[e_sd] + pr)
        ts, tt, tfree = ctx.t.next()
        hs, hb, hfree = ctx.hb.next()
        e_t = vstt(tt[:], xin, rstd[:, col:col + 1], gsc_t[:], ALU.mult, ALU.mult, waits=list(waits) + tfree + pr + [e_r])
        if h32 is not None:
            vtt(h32[:], tt[:], sh_t[:], ALU.add)
            e_h = vcopy(hb[:], h32[:], waits=hfree)
        else:
            e_h = vtt(hb[:], tt[:], sh_t[:], ALU.add, waits=hfree)
        ctx.t.release(ts, [e_h])
        ps, pt, pfree = ctx.pt.next()
        e_tr = None
        for k in range(8):
            e_tr = tr(pt[:, k * 128:(k + 1) * 128], hb[:, k * 128:(k + 1) * 128], ident[:],
                      waits=[e_h] + pfree + pr, signal=(k == 7))
        ctx.hb.release(hs, [e_tr])
        e_c = act(hT_dst, pt[:, :].rearrange("p (k t) -> p k t", k=8), AF.Copy, waits=[e_tr])
        ctx.pt.release(ps, [e_c])
        return e_c, e_t, e_sq

    e_eps = memset(small[:, 0:1], 1e-6, eng='dve')
    e_one = memset(small[:, 1:2], 1.0, eng='dve')

    e_mod0 = compute_mods(0, B0, [])

    hT = T("hT", [128, 8, S], BF16, 0)
    PT = [T("PT", [128, 384], BF16, 65536 + i * 768) for i in range(4)]
    selk_t = [T("selk", [80, 71], BF16, 68608 + i * 160) for i in range(2)]
    selq_t = [T("selq", [80, 71], BF16, 68928 + i * 160) for i in range(2)]
    oT = T("oT", [128, 8, TOWN], BF16, B0)
    o_pair = [T("opair", [128, NOWN, 128], BF16, B0 + 34816 + i * 4352) for i in range(2)]
    V_grp = T("Vgrp", [128, 32, 4, 65], BF16, B0 + 43520)
    Fsrc = T("Fsrc", [80, S], BF16, B0 + 60160)
    KT = [T("KT", [71, S], BF16, B0 + 68352 + i * 8192) for i in range(2)]
    QT = [T("QT", [71, TOWN], BF16, B0 + 84736 + i * 4352) for i in range(2)]
    Wqk = [T("Wqk", [128, 8, 142], BF16, B0 + 93440 + i * 2272) for i in range(2)]
    Wv = T("Wv", [128, 8, 256], BF16, B0 + 97984)
    Wf = T("Wf", [128, 8, 16], BF16, B0 + 102080)
    NB = B0 + 68352
    xin_bufs = [T("xin", [128, D], F32, NB + 14336 + i * 4096) for i in range(2)]

    nctx = NormCtx(NB, [e_mod0])
    xring = Ring(xin_bufs)
    ds_x = [DmaSem(P, f"dx{i}") for i in range(2)]
    e_hT = None
    for i in range(32):
        src = xc_d[i * 128:(i + 1) * 128, :] if i < NCTX else xo_d[(i - NCTX) * 128:(i - NCTX + 1) * 128, :]
        xs, xb, xfree = xring.next()
        e_ld = P.dma('sp', ds_x[xs], xb[:], src, waits=xfree + [e_mod0])
        e_c, e_t, e_sq = norm_block(nctx, xb[:], i, modt[GSC1], modt[SH1], hT[:, :, i * 128:(i + 1) * 128],
                                    [e_ld, e_mod0, e_eps])
        xring.release(xs, [e_t, e_sq])
        e_hT = e_c

    if stage == 1:
        e1 = P.dma('sp', ds_out, dbg2_d, hT[:, :, :].rearrange("p k t -> p (k t)"), waits=[e_hT])
        P.wait_only('sp', [e1])
        P.emit()
        P.close()
        return nc


    X1 = T("X1", [H, S], F32, B0)
    X2 = T("X2", [H, 2048], F32, B0 + 16384)
    X3 = T("X3", [H, 2048], F32, B0 + 24576)
    thi = T("thi", [H, 2048], BF16, B0 + 32768)
    tmid = T("tmid", [H, 2048], BF16, B0 + 36864)
    tlo = T("tlo", [H, 2048], BF16, B0 + 40960)
    wvv = fwin_d[0].rearrange("(k p) n -> p k n", p=128)
    ds_wf = DmaSem(P, "dwf")
    e_wf = P.dma('pool', ds_wf, Wf[:], wvv[:, :, 3 * D:3 * D + H])
    e_fz = memset(Fsrc[:], 0.0, eng='pool')
    ds_kb = DmaSem(P, "dkb")
    e_kb = P.dma('sp', ds_kb, Fsrc[16:18, :], kb2_d, waits=[e_fz])
    ds_f = DmaSem(P, "dfs")
    projring = Ring([pb[6], pb[7]])
    projringb = {id(pb[6]): pbb[6], id(pb[7]): pbb[7]}
    e_prev = None
    e_fdma = []
    for c in range(2):
        sl = slice(c * 2048, (c + 1) * 2048)
        for t4 in range(4):
            tg = c * 4 + t4
            ps_, pt, pfree = projring.next()
            e_m = None
            for k in range(8):
                e_m = mm(pt[0:H, :], Wf[:, k, :], hT[:, k, tg * 512:(tg + 1) * 512], k == 0, k == 7,
                         waits=[e_wf, e_hT] + pfree, signal=(k == 7))
            e_z = act(X1[:, tg * 512:(tg + 1) * 512], pt[0:H, :], AF.Identity, waits=[e_m] + e_fdma, bias=bf_col[:, 0:1])
            projring.release(ps_, [e_z])
        e = act(X2[:], X1[:, sl], AF.Abs, waits=[e_z] + e_fdma)
        e = act(X2[:], X2[:], AF.Exp, waits=[e], scale=-1.0)
        e = act(X2[:], X2[:], AF.Ln, waits=[e, e_one], bias=small[0:H, 1:2])
        e = vstt(X3[:], X1[:, sl], 0.0, X2[:], ALU.min, ALU.subtract, waits=[e] + e_fdma)
        e = memset(X2[:], 1.0, waits=[e], eng='dve')
        init = 0.0 if c == 0 else X1[:, c * 2048 - 1:c * 2048]
        e = P.op('dve', lambda en, sl=sl, init=init: en.tensor_tensor_scan(
            out=X1[:, sl], data0=X2[:], data1=X3[:], initial=init, op0=ALU.mult, op1=ALU.add), waits=[e, e_prev],
            reads=_aps(X2[:], X3[:], init), writes=[X1[:, sl]])
        e_prev = e
        e1 = act(thi[:], X1[:, sl], AF.Identity, waits=[e] + e_fdma, scale=8.0)
        e = vstt(X3[:], X1[:, sl], 8.0, thi[:], ALU.mult, ALU.subtract, waits=[e1])
        e2 = act(tmid[:], X3[:], AF.Copy, waits=[e] + e_fdma)
        e = vtt(X2[:], X3[:], tmid[:], ALU.subtract, waits=[e2])
        e3 = act(tlo[:], X2[:], AF.Copy, waits=[e] + e_fdma)
        e_fdma = [P.dma('sp', ds_f, Fsrc[0:16, sl], thi[:], waits=[e1, e_fz]),
                  P.dma('sp', ds_f, Fsrc[32:48, sl], tmid[:], waits=[e2]),
                  P.dma('sp', ds_f, Fsrc[64:80, sl], tlo[:], waits=[e3])]
    e_F = [e_fdma[-1], e_kb]

    if stage == 15:
        e1 = P.dma('sp', ds_out, dbg3_d[0:H, :], X1[:], waits=e_F)
        e2 = P.dma('sp', ds_out, dbg2_d[0:80, 0:S], Fsrc[:], waits=e_F)
        P.wait_only('sp', [e2])
        P.emit()
        P.close()
        return nc

    ds_qk = [DmaSem(P, f"dqk{i}") for i in range(2)]
    ds_sel = [DmaSem(P, f"dsel{i}") for i in range(2)]
    ds_wv = DmaSem(P, "dwv")
    sring = Ring([pb[0], pb[1], pb[5]])
    ptring = Ring(PT)
    Obank = [pb[2], pb[3], pb[4]]
    Ofree = [[], [], []]
    SBS = [(0, 2), (2, 5), (5, 8), (8, 11), (11, 14), (14, 17)]
    QTG = [(0, 512), (512, 512), (1024, 512), (1536, 512), (2048, 128)]

    def head_tiles():
        tl = []
        for (i0, i1) in SBS:
            nq = i1 - i0
            for kb in range(0, NCTX + i1):
                j0 = max(0, kb - NCTX - i0)
                tl.append((i0, nq, kb, j0, kb >= NCTX + i0))
        return tl
    TILES = head_tiles()

    state = {"proj_done": {}, "kq_ready": {}, "wload": {}, "v_ready": None, "v_last_read": [e_fdma[-1]],
             "last_pv": None}

    def load_head_w(h):
        slot = h % 2
        w = state["proj_done"].get(h - 2, [])
        if h < 2:
            memset(Wqk[slot][:, :, 64:71], 0.0, eng='pool')
            memset(Wqk[slot][:, :, 135:142], 0.0, eng='pool')
        P.dma('pool', ds_qk[slot], Wqk[slot][:, :, 0:64], wvv[:, :, h * 64:(h + 1) * 64], waits=w)
        e1 = P.dma('pool', ds_qk[slot], Wqk[slot][:, :, 71:135], wvv[:, :, D + h * 64:D + (h + 1) * 64], waits=w)
        P.dma('sp', ds_sel[slot], selk_t[slot][:], selk_d[h], waits=w)
        e2 = P.dma('sp', ds_sel[slot], selq_t[slot][:], selq_d[h], waits=w)
        state["wload"][h] = [e1, e2]

    def proj_units(h):
        slot = h % 2
        units = []
        evs = []
        state["kq_ready"][h] = evs

        def unit(kind, c0, n, dst):
            def f():
                ps_, pt, pfree = projring.next()
                wl = state["wload"][h] + e_F + pfree
                if kind == 'k':
                    mm(pt[0:71, 0:n], selk_t[slot][:, :], Fsrc[:, c0:c0 + n], True, False, waits=wl)
                    wc = slice(71, 142)
                else:
                    mm(pt[0:71, 0:n], selq_t[slot][:, :], Fsrc[:, c0:c0 + n], True, False, waits=wl)
                    wc = slice(0, 71)
                e_m = None
                for k in range(8):
                    e_m = mm(pt[0:71, 0:n], Wqk[slot][:, k, wc], hT[:, k, c0:c0 + n], False, k == 7, signal=(k == 7))
                e_c = vcopy(dst, pt[0:71, 0:n], waits=[e_m])
                projring.release(ps_, [e_c])
                evs.append(e_c)
                state["proj_done"][h] = [e_m]
            return f
        for tg in range(8):
            units.append(unit('k', tg * 512, 512, KT[slot][:, tg * 512:(tg + 1) * 512]))
        for (st, n) in QTG:
            units.append(unit('q', TCTX + st, n, QT[slot][:, st:st + n]))
        return units

    def compute_V(g):
        w = state["v_last_read"]
        e_w = P.dma('pool', ds_wv, Wv[:], wvv[:, :, 2 * D + g * 256:2 * D + (g + 1) * 256], waits=w)
        evs = []
        if g == 0:
            evs.append(memset(V_grp[:, :, :, 64:65], 1.0, waits=w, eng='pool'))
        for tb2 in range(16):
            ps_, pt, pfree = projring.next()
            e_m = None
            for j in range(2):
                tb = tb2 * 2 + j
                for k in range(8):
                    e_m = mm(pt[:, j * 256:(j + 1) * 256], hT[:, k, tb * 128:(tb + 1) * 128], Wv[:, k, :], k == 0, k == 7,
                             waits=[e_w] + pfree, signal=(j == 1 and k == 7))
            src = pt[:, :].rearrange("p (j h d) -> p j h d", j=2, h=4)
            dst = V_grp[:, tb2 * 2:tb2 * 2 + 2, :, 0:64]
            e_c = vcopy(dst, src, waits=[e_m] + w)
            projring.release(ps_, [e_c])
            evs.append(e_c)
        state["v_ready"] = evs
        state["v_last_read"] = [e_m]

    pend = []

    def emit_S(h, tile):
        i0, nq, kb, j0, diag = tile
        slot = h % 2
        ss, sb_, sfree = sring.next()
        c0, c1 = j0 * 128, nq * 128
        e_s = mm(sb_[:, c0:c1], KT[slot][:, kb * 128:(kb + 1) * 128], QT[slot][:, i0 * 128 + c0:i0 * 128 + c1],
                 True, not diag, waits=state["kq_ready"][h] + sfree, signal=not diag)
        if diag:
            e_s = mm(sb_[:, c0:c0 + 128], ident[:], trim[:], False, True, signal=True)
        return (h, tile, ss, sb_, e_s)

    def emit_EXP_PV(rec):
        h, tile, ss, sb_, e_s = rec
        i0, nq, kb, j0, diag = tile
        c0, c1 = j0 * 128, nq * 128
        ps_, ptb, pfree = ptring.next()
        e_x = act(ptb[:, c0:c1], sb_[:, c0:c1], AF.Exp, waits=[e_s] + pfree, scale=0.125)
        sring.release(ss, [e_x])
        hh = h % 4
        e_pv = None
        for j in range(j0, nq):
            first = (kb == 0)
            last = (kb == NCTX + i0 + j)
            wl = [e_x] + state["v_ready"]
            if first:
                wl = wl + Ofree[j]
            is_last_of_tile = (j == nq - 1)
            e_pv = mm(Obank[j][:, 0:65], ptb[:, j * 128:(j + 1) * 128], V_grp[:, kb, hh, :], first, last,
                      waits=wl, signal=(last or is_last_of_tile))
            if last:
                col = 8 + j
                vts(small[:, col:col + 1], Obank[j][:, 64:65], 1e-30, None, ALU.max, waits=[e_pv])
                e_r = vrecip(small[:, col:col + 1], small[:, col:col + 1])
                pslot = (h // 2) % 2
                e_n = vts(o_pair[pslot][:, i0 + j, hh % 2 * 64:hh % 2 * 64 + 64], Obank[j][:, 0:64],
                          small[:, col:col + 1], None, ALU.mult, waits=[e_r] + state.get("opair_free%d" % pslot, []))
                Ofree[j] = [e_n]
                state["last_norm"] = e_n
        ptring.release(ps_, [e_pv])
        state["last_pv"] = e_pv

    def emit_opair_T(h):
        pair = h // 2
        pslot = pair % 2
        e_last = None
        for i0_ in range(0, NOWN, 8):
            n = min(8, NOWN - i0_)
            ps_, pt, pfree = projring.next()
            ptv = projringb[id(pt)]
            e_t = None
            for i in range(n):
                e_t = tr(ptv[:, i * 128:(i + 1) * 128], o_pair[pslot][:, i0_ + i, :], ident[:],
                         waits=[state["last_norm"]] + pfree, signal=(i == n - 1))
            e_c = vcopy(oT[:, pair, i0_ * 128:(i0_ + n) * 128], ptv[:, 0:n * 128], waits=[e_t])
            projring.release(ps_, [e_c])
            e_last = e_t
        state["opair_free%d" % pslot] = [e_last]
        state["oT_last"] = e_c

    load_head_w(0)
    compute_V(0)
    for u in proj_units(0):
        u()
    for h in range(H):
        nxt = []
        if h + 1 < H:
            load_head_w(h + 1)
            if (h + 1) % 4 != 0:
                nxt = proj_units(h + 1)
        recs = [emit_S(h, TILES[0]), emit_S(h, TILES[1])]
        nt = len(TILES)
        every = max(1, nt // (len(nxt) + 1)) if nxt else nt + 1
        for t in range(nt):
            if t + 2 < nt:
                recs.append(emit_S(h, TILES[t + 2]))
            emit_EXP_PV(recs[t])
            if nxt and (t % every == every - 1):
                nxt.pop(0)()
        while nxt:
            nxt.pop(0)()
        if h % 2 == 1:
            emit_opair_T(h)
        if h + 1 < H and (h + 1) % 4 == 0:
            state["v_last_read"] = [state["last_pv"]]
            compute_V((h + 1) // 4)
            for u in proj_units(h + 1):
                u()

    if stage == 2:
        e1 = P.dma('sp', ds_out, dbg2_d[:, 0:8 * TOWN], oT[:, :, :].rearrange("p k t -> p (k t)"), waits=[state["oT_last"]])
        P.wait_only('sp', [e1])
        P.emit()
        P.close()
        return nc


    def finish_dbg():
        e1 = P.dma('sp', ds_out, dbg_d, x_res[:, :, :].rearrange("p i d -> p (i d)"))
        P.wait_only('sp', [e1])
        P.emit()
        P.close()
        return nc

    def out_proj(w_src, name):
        Wo = T("Wo" + name, [128, 8, D], BF16, B0 + 93440)
        ytmp = [T("ytmp" + name, [128, 512], F32, B0 + 109824 + i * 2048) for i in range(2)]
        ds_wo = DmaSem(P, "dwo" + name)
        wsrc = w_src.rearrange("(k p) n -> p k n", p=128)
        for k2 in range(2):
            P.dma('pool', ds_wo, Wo[:, :, k2 * 512:(k2 + 1) * 512], wsrc[:, :, k2 * 512:(k2 + 1) * 512])
        return Wo, ytmp

    Wo, ytmp = out_proj(fwout_d[0], "f")
    ds_xr = DmaSem(P, "dxr")
    for i in range(NOWN):
        P.dma('sp', ds_xr, x_res[:, i, :], xo_d[i * 128:(i + 1) * 128, :])
    n_y = 0
    for i in range(NOWN):
        for half in range(2):
            bank = pb[n_y % 2]
            yt = ytmp[n_y % 2]
            n_y += 1
            hs = slice(half * 512, (half + 1) * 512)
            for k in range(8):
                mm(bank[:, :], oT[:, k, i * 128:(i + 1) * 128], Wo[:, k, hs], k == 0, k == 7)
            vtt(yt[:], bank[:], modt[GT1][:, hs], ALU.mult)
            vtt(x_res[:, i, hs], x_res[:, i, hs], yt[:], ALU.add, eng='pool')

    if stage == 3:
        return finish_dbg()

    FB = B0
    hT2 = T("hT2", [128, 8, TOWN], BF16, FB)
    actT = [T("actT", [128, 4, 512], BF16, FB + 34816 + i * 4096) for i in range(2)]
    wgu_t = [T("wgu", [128, 8, 1024], BF16, FB + 43008 + i * 16384) for i in range(2)]
    wd_t = [T("wd", [128, 4, D], BF16, FB + 75776 + i * 8192) for i in range(2)]
    sg_t = [T("sg", [128, 512], F32, FB + 92160 + i * 2048) for i in range(2)]
    NB2 = FB + 96256
    gates = T("gates", [128, NOWN * NE], F32, FB + 110592)
    ds_gu = [DmaSem(P, f"dgu{i}") for i in range(2)]
    ds_wd = [DmaSem(P, f"dwd{i}") for i in range(2)]
    ffn_state = {"stage": 0, "g": 0, "u": 0, "y": 0, "a": 0, "s": 0}
    Gb = [pb[0], pb[1]]
    Ub = [pb[2], pb[3]]
    Yb = [pb[4], pb[5]]

    def ffn_pass(gsrc, usrc, dsrc, blk0, blk1, gt_tile, expert=None):
        groups = [(b, min(b + 4, blk1)) for b in range(blk0, blk1, 4)]
        st = ffn_state

        def emit_down(aslot, b0, b1, slot):
            for b in range(b0, b1):
                for half in range(2):
                    Y = Yb[st["y"] % 2]
                    st["y"] += 1
                    hs = slice(half * 512, (half + 1) * 512)
                    for fc in range(4):
                        mm(Y[:, :], actT[aslot][:, fc, (b - b0) * 128:(b - b0 + 1) * 128], wd_t[slot][:, fc, hs], fc == 0, fc == 3)
                    if expert is None:
                        vtt(x_res[:, b, hs], Y[:, :], x_res[:, b, hs], ALU.add)
                    else:
                        vstt(x_res[:, b, hs], Y[:, :], gates[:, b * NE + expert:b * NE + expert + 1], x_res[:, b, hs],
                             ALU.mult, ALU.add)

        for fg in range(7):
            slot = st["stage"] % 2
            st["stage"] += 1
            fs = slice(fg * 512, (fg + 1) * 512)
            P.dma('pool', ds_gu[slot], wgu_t[slot][:, :, 0:512], gsrc[:, :, fs])
            P.dma('pool', ds_gu[slot], wgu_t[slot][:, :, 512:1024], usrc[:, :, fs])
            P.dma('pool', ds_wd[slot], wd_t[slot][:, :, :], dsrc[fg * 512:(fg + 1) * 512, :].rearrange("(c p) n -> p c n", p=128))
            for c in range(4):
                vtt(wd_t[slot][:, c, :], wd_t[slot][:, c, :], gt_tile[:], ALU.mult, eng='pool')
            pending = None
            for (b0, b1) in groups:
                ntok = (b1 - b0) * 128
                t0 = b0 * 128
                aslot = st["a"] % 2
                st["a"] += 1
                for fc in range(4):
                    G = Gb[st["g"] % 2]
                    U = Ub[st["g"] % 2]
                    st["g"] += 1
                    sg = sg_t[st["s"] % 2]
                    st["s"] += 1
                    for k in range(8):
                        mm(G[:, 0:ntok], wgu_t[slot][:, k, fc * 128:(fc + 1) * 128], hT2[:, k, t0:t0 + ntok], k == 0, k == 7)
                    for k in range(8):
                        mm(U[:, 0:ntok], wgu_t[slot][:, k, 512 + fc * 128:512 + (fc + 1) * 128], hT2[:, k, t0:t0 + ntok],
                           k == 0, k == 7)
                    act(sg[:, 0:ntok], G[:, 0:ntok], AF.Silu)
                    vtt(actT[aslot][:, fc, 0:ntok], sg[:, 0:ntok], U[:, 0:ntok], ALU.mult)
                if pending is not None:
                    emit_down(*pending)
                pending = (aslot, b0, b1, slot)
            emit_down(*pending)

    nctx2 = NormCtx(NB2, [])
    for i in range(NOWN):
        norm_block(nctx2, x_res[:, i, :], 32 + i, modt[GSC2], modt[SH2], hT2[:, :, i * 128:(i + 1) * 128], [])
    gu0 = wgu_d[0].rearrange("(k p) n -> p k n", p=128)
    ffn_pass(gu0[:, :, 0:FF], gu0[:, :, FF:2 * FF], wdn_d[0], 0, NOWN, modt[GT2])

    if stage == 4:
        return finish_dbg()

    e_mod1 = compute_mods(1, FB + 43008, [])

    nctx3 = NormCtx(NB2, [])
    for i in range(NOWN):
        norm_block(nctx3, x_res[:, i, :], i, modt[GSC1], modt[SH1], hT2[:, :, i * 128:(i + 1) * 128], [])
    KT2 = T("KT2", [128, 4, TOWN], BF16, FB + 34816)
    V2 = T("V2", [128, NOWN, 4, 65], BF16, FB + 52224)
    QT2 = T("QT2", [128, 8, 2048], BF16, FB + 61088)
    swab_t = T("swab", [128, 4 * 2 * 4 * 128], BF16, FB + 93856)
    wst = T("wst", [128, 8, 512], BF16, FB + 93856)
    PT2 = [T("PT2", [128, 512], BF16, FB + 102048 + i * 1024) for i in range(4)]
    otok = [T("otok", [128, D], BF16, FB + 106144 + i * 2048) for i in range(2)]
    oTb = [T("oTb", [128, 8, 128], BF16, FB + 110240 + i * 2048) for i in range(2)]
    Wo2 = T("Wo2", [128, 8, D], BF16, FB)
    ytmp2 = [T("ytmp2", [128, 512], F32, FB + 16384 + i * 2048) for i in range(2)]
    swv = swin_d[0].rearrange("(k p) n -> p k n", p=128)
    ds_ws = DmaSem(P, "dws")
    for g in range(4):
        for dup in range(2):
            P.dma('pool', ds_ws, wst[:, :, g * 128 + dup * 64:g * 128 + dup * 64 + 64], swv[:, :, D + g * 64:D + (g + 1) * 64])
    npj = 0
    for (st_, n) in [(0, 512), (512, 512), (1024, 512), (1536, 512), (2048, 128)]:
        for g in range(4):
            pt = pb[6 + npj % 2]
            npj += 1
            for k in range(8):
                mm(pt[:, 0:n], wst[:, k, g * 128:(g + 1) * 128], hT2[:, k, st_:st_ + n], k == 0, k == 7)
            if npj % 2 == 0:
                act(KT2[:, g, st_:st_ + n], pt[:, 0:n], AF.Copy)
            else:
                vcopy(KT2[:, g, st_:st_ + n], pt[:, 0:n])
    memset(V2[:, :, :, 64:65], 1.0, eng='pool')
    P.dma('pool', ds_ws, wst[:, :, 0:256], swv[:, :, D + 256:D + 512])
    for i in range(NOWN):
        pt = pb[6 + npj % 2]
        npj += 1
        for k in range(8):
            mm(pt[:, 0:256], hT2[:, k, i * 128:(i + 1) * 128], wst[:, k, 0:256], k == 0, k == 7)
        src = pt[:, 0:256].rearrange("p (g d) -> p g d", g=4)
        if npj % 2 == 0:
            act(V2[:, i, :, 0:64], src, AF.Copy)
        else:
            vcopy(V2[:, i, :, 0:64], src)
    for qh in range(2):
        P.dma('pool', ds_ws, wst[:, :, :], swv[:, :, qh * 512:(qh + 1) * 512])
        for pr_ in range(4):
            pair = qh * 4 + pr_
            for tg in range(4):
                pt = pb[6 + npj % 2]
                npj += 1
                for k in range(8):
                    mm(pt[:, :], wst[:, k, pr_ * 128:(pr_ + 1) * 128], hT2[:, k, 128 + tg * 512:128 + (tg + 1) * 512], k == 0, k == 7)
                act(QT2[:, pair, tg * 512:(tg + 1) * 512], pt[:, :], AF.Identity, scale=0.125)
    ds_w2 = DmaSem(P, "dw2")
    wo2src = swout_d[0].rearrange("(k p) n -> p k n", p=128)
    for k2 in range(2):
        P.dma('pool', ds_w2, Wo2[:, :, k2 * 512:(k2 + 1) * 512], wo2src[:, :, k2 * 512:(k2 + 1) * 512])
    ds_swab = DmaSem(P, "dswab")
    P.dma('sp', ds_swab, swab_t[:], swab_d)
    n_pt = 0
    n_y = 0
    swa_ofree = [[], []]
    for i in range(1, NOWN):
        ot = otok[i % 2]
        for g in range(4):
            pts = []
            npair = (i * 4 + g) % 2
            for p_ in range(2):
                Sb = pb[2 * npair + p_]
                kblk = i - 1 + p_
                for hd in range(4):
                    h = 4 * g + hd
                    pair, par = h // 2, h % 2
                    b0_ = (g * 2 + p_) * 512 + hd * 128
                    mm(Sb[:, hd * 128:(hd + 1) * 128], ident[:], swab_t[:, b0_:b0_ + 128], True, False)
                    mm(Sb[:, hd * 128:(hd + 1) * 128], KT2[par * 64:(par + 1) * 64, g, kblk * 128:(kblk + 1) * 128],
                       QT2[par * 64:(par + 1) * 64, pair, (i - 1) * 128:i * 128], False, True)
                ptb = PT2[n_pt % 4]
                n_pt += 1
                if i == 1 and p_ == 0:
                    act(ptb[:, :], Sb[:, :], AF.Exp, bias=halo_t[:, 0:1])
                else:
                    act(ptb[:, :], Sb[:, :], AF.Exp)
                pts.append(ptb)
            Ob = pb[4 + npair]
            e_pv = None
            for hd in range(4):
                oc = hd * 128
                for p_ in range(2):
                    e_pv = mm(Ob[:, oc:oc + 65], pts[p_][:, hd * 128:(hd + 1) * 128], V2[:, i - 1 + p_, g, :], p_ == 0, p_ == 1,
                              waits=swa_ofree[npair])
            Obv = Ob[:, :].rearrange("p (h c) -> p h c", c=128)
            c0_ = 16 + 4 * npair
            den = small[:, c0_:c0_ + 4]
            vtt(den, Obv[:, :, 64], esink[:, 4 * g:4 * g + 4], ALU.add, waits=[e_pv])
            vrecip(den, den)
            denb = den.rearrange("p (a o) -> p a o", o=1).broadcast_to([128, 4, 64])
            e_n = vtt(ot[:, g * 256:(g + 1) * 256].rearrange("p (h d) -> p h d", h=4), Obv[:, :, 0:64], denb, ALU.mult)
            swa_ofree[npair] = [e_n]
        ob = oTb[i % 2]
        ptv = pbb[6]
        for k in range(8):
            tr(ptv[:, k * 128:(k + 1) * 128], ot[:, k * 128:(k + 1) * 128], ident[:])
        act(ob[:, :, :], ptv[:, :].rearrange("p (k t) -> p k t", k=8), AF.Copy)
        for half in range(2):
            yt = ytmp2[n_y % 2]
            n_y += 1
            hs = slice(half * 512, (half + 1) * 512)
            for k in range(8):
                mm(pb[7][:, :], ob[:, k, :], Wo2[:, k, hs], k == 0, k == 7)
            vtt(yt[:], pb[7][:, :], modt[GT1][:, hs], ALU.mult)
            vtt(x_res[:, i, hs], x_res[:, i, hs], yt[:], ALU.add, eng='pool')

    if stage == 5:
        return finish_dbg()

    h32s = [T("h32", [128, D], F32, FB + 43008 + i * 8448) for i in range(2)]
    hT32s = [T("hT32", [128, 8, 128], F32, FB + 43008 + 4096 + i * 8448) for i in range(2)]
    wr32 = T("wr32", [128, 8, NE], F32, FB + 43008 + 16896)
    RB = FB + 43008 + 17152
    LA = T("LA", [128, 16, NE], F32, RB)
    EQ1 = T("EQ1", [128, 16, NE], F32, RB + 512)
    L2 = T("L2", [128, 16, NE], F32, RB + 1024)
    EQ2 = T("EQ2", [128, 16, NE], F32, RB + 1536)
    M1 = T("M1", [128, 16], F32, RB + 2048)
    M2 = T("M2", [128, 16], F32, RB + 2112)
    Dm = T("Dm", [128, 16], F32, RB + 2176)
    Ed = T("Ed", [128, 16], F32, RB + 2240)
    W1 = T("W1", [128, 16], F32, RB + 2304)
    W2 = T("W2", [128, 16], F32, RB + 2368)
    ds_wr = DmaSem(P, "dwr")
    P.dma('sp', ds_wr, wr32[:], wr_d[0].rearrange("(k p) n -> p k n", p=128))
    nctx4 = NormCtx(NB2, [])
    for i in range(1, NOWN):
        h32, hT32 = h32s[i % 2], hT32s[i % 2]
        norm_block(nctx4, x_res[:, i, :], 32 + i, modt[GSC2], modt[SH2], hT2[:, :, i * 128:(i + 1) * 128], [], h32=h32)
        for half in range(2):
            pt = pb[half + 2 * (i % 2)]
            for k4 in range(4):
                k = half * 4 + k4
                tr(pt[:, k4 * 128:(k4 + 1) * 128], h32[:, k * 128:(k + 1) * 128], identf[:])
            vcopy(hT32[:, half * 4:half * 4 + 4, :], pt[:, :].rearrange("p (k t) -> p k t", k=4))
        lgb = pb[4 + i % 2]
        for k in range(8):
            mm(lgb[:, 0:NE], hT32[:, k, :], wr32[:, k, :], k == 0, k == 7)
        vtt(LA[:, i - 1, :], lgb[:, 0:NE], brb[:], ALU.add)

    def bc(v):
        return v.rearrange("p (a o) -> p a o", o=1).broadcast_to([128, 16, NE])
    P.op('dve', lambda e: e.tensor_reduce(out=M1[:], in_=LA[:], axis=AX.X, op=ALU.max), reads=[LA[:]], writes=[M1[:]])
    vtt(EQ1[:], LA[:], bc(M1[:]), ALU.is_equal)
    vstt(L2[:], EQ1[:], -1e30, LA[:], ALU.mult, ALU.add)
    P.op('dve', lambda e: e.tensor_reduce(out=M2[:], in_=L2[:], axis=AX.X, op=ALU.max), reads=[L2[:]], writes=[M2[:]])
    vtt(EQ2[:], L2[:], bc(M2[:]), ALU.is_equal)
    vtt(Dm[:], M2[:], M1[:], ALU.subtract)
    act(Ed[:], Dm[:], AF.Exp)
    vts(W1[:], Ed[:], 1.0, None, ALU.add)
    vrecip(W1[:], W1[:])
    vtt(W2[:], Ed[:], W1[:], ALU.mult)
    vtt(EQ1[:], EQ1[:], bc(W1[:]), ALU.mult)
    vtt(EQ2[:], EQ2[:], bc(W2[:]), ALU.mult)
    vtt(gates[:, NE:NOWN * NE].rearrange("p (a b) -> p a b", b=NE), EQ1[:], EQ2[:], ALU.add)

    for e_ in range(NE):
        mg = mgu_d[0, e_].rearrange("(k p) n -> p k n", p=128)
        ffn_pass(mg[:, :, 0:FF], mg[:, :, FF:2 * FF], mdn_d[0, e_], 1, NOWN, modt[GT2], expert=e_)

    if stage == 6:
        return finish_dbg()

    gfin = modt[SH1]
    ds_gf = DmaSem(P, "dgf")
    P.dma('sp', ds_gf, gfin[:], gfin_d[0:1, :].partition_broadcast(128))
    fo = [T("fo", [128, D], F32, NB2 + i * 4096) for i in range(2)]
    fjunk = T("fjunk", [128, D], BF16, NB2 + 8192)
    last = []
    for i in range(1, NOWN):
        col = i
        act(fjunk[:], x_res[:, i, :], AF.Square, accum_out=ssq[:, col:col + 1])
        act(rstd[:, col:col + 1], ssq[:, col:col + 1], AF.Sqrt, scale=1.0 / D, bias=small[:, 0:1])
        vrecip(rstd[:, col:col + 1], rstd[:, col:col + 1])
        f = fo[i % 2]
        vstt(f[:], x_res[:, i, :], rstd[:, col:col + 1], gfin[:], ALU.mult, ALU.mult)
        last.append(P.dma('sp', ds_out, out_d[(i - 1) * 128:i * 128, :], f[:]))
    P.wait_only('sp', last)
    P.emit()
    P.close()
    return nc


def t5_band_buckets():
    CHUNK, BAND, WC = 64, 192, 2
    q = np.arange(CHUNK)[:, None]
    k = np.arange(BAND)[None, :] - WC * CHUNK
    rel = k - q
    nb = 16
    max_exact = nb // 2
    ret = (rel > 0).astype(np.int32) * nb
    n = np.abs(rel)
    large = max_exact + (np.log(np.maximum(n, 1) / max_exact)
                         / np.log(128 / max_exact) * (nb - max_exact)).astype(np.int32)
    large = np.minimum(large, nb - 1)
    return (ret + np.where(n < max_exact, n, large)).astype(np.int32)


def make_inputs(inputs, stage=99):
    bf = ml_dtypes.bfloat16
    x = np.asarray(inputs["x"], dtype=np.float32)
    c = np.asarray(inputs["c"], dtype=np.float32)
    ident = np.eye(128, dtype=np.float32)
    kk = np.arange(128)[:, None]
    qq = np.arange(128)[None, :]
    trimask = np.where(kk > qq, NEG, 0.0).astype(bf)
    selk = np.zeros((H, 80, 71), np.float32)
    selq = np.zeros((H, 80, 71), np.float32)
    for h in range(H):
        selk[h, h, 67] = -1
        selk[h, 32 + h, 68] = -1
        selk[h, 64 + h, 69] = -1
        selk[h, 16, 64:67] = 1
        selk[h, 17, 70] = 1
        selq[h, h, 64] = 1
        selq[h, 32 + h, 65] = 1
        selq[h, 64 + h, 66] = 1
        selq[h, 16, 67:71] = 1
    bk = t5_band_buckets()
    rel_bias = np.asarray(inputs["rel_bias"], dtype=np.float32)
    idx = np.full((256, 128), -1, np.int64)
    for q in range(128):
        for kl in range(256):
            if q < 64:
                if kl < 192:
                    idx[kl, q] = bk[q, kl]
            else:
                if kl >= 64:
                    idx[kl, q] = bk[q - 64, kl - 64]
    swab = np.full((128, 4, 2, 4, 128), NEG, np.float32)
    valid = idx >= 0
    for g in range(4):
        for hd in range(4):
            h = g * 4 + hd
            full = np.where(valid, rel_bias[np.maximum(idx, 0), h], NEG)
            swab[:, g, 0, hd, :] = full[0:128]
            swab[:, g, 1, hd, :] = full[128:256]
    swab = swab.reshape(128, -1).astype(bf)

    shared = {
        "ident": ident.astype(bf), "identf": ident, "trimask": trimask,
        "selk": selk.astype(bf), "selq": selq.astype(bf), "swab": swab,
        "w_ada": inputs["w_ada"], "b_ada": inputs["b_ada"],
        "g_norm_mix": inputs["g_norm_mix"], "g_norm_ffn": inputs["g_norm_ffn"],
        "g_final": np.asarray(inputs["g_final"]).reshape(1, D),
        "fox_w_in": inputs["fox_w_in"], "fox_b_f": np.asarray(inputs["fox_b_f"]).reshape(H, 1),
        "fox_w_out": inputs["fox_w_out"], "swa_w_in": inputs["swa_w_in"],
        "swa_sinks": inputs["swa_sinks"], "swa_w_out": inputs["swa_w_out"],
        "ffn_w_gu": inputs["ffn_w_gu"], "ffn_w_down": inputs["ffn_w_down"],
        "moe_w_router": inputs["moe_w_router"], "moe_b_router": inputs["moe_b_router"],
        "moe_w_gu": inputs["moe_w_gu"], "moe_w_down": inputs["moe_w_down"],
    }
    if stage < 6:
        del shared["moe_w_gu"], shared["moe_w_down"]
    shared = {k: np.ascontiguousarray(np.asarray(v)) for k, v in shared.items()}
    in_maps = []
    for core in range(8):
        b, half = core // 2, core % 2
        xo = np.zeros((TOWN, D), np.float32)
        xc = np.zeros((TCTX, D), np.float32)
        kb2 = np.zeros((2, S), np.float32)
        kb2[0, :] = 1.0
        halo = np.zeros((128, 1), np.float32)
        if half == 0:
            xo[128:] = x[b, 0:2048]
            kb2[1, 0:TCTX + 128] = NEG
            halo[:] = NEG
        else:
            xo[:] = x[b, TCTX:S]
            xc[:] = x[b, 0:TCTX]
        m = dict(shared)
        m["xo"] = xo
        m["xc"] = xc
        m["cT"] = np.ascontiguousarray(c[b].reshape(8, 128).T)
        m["kb2"] = kb2.astype(bf)
        m["halo"] = halo
        in_maps.append(m)
    return in_maps


_CACHE = {}


def kernel(**inputs):
    stage = int(os.environ.get("KSTAGE", "99"))
    if stage not in _CACHE:
        _CACHE[stage] = build_program(stage)
    nc = _CACHE[stage]
    in_maps = make_inputs(inputs, stage)
    res = run_bass_kernel_spmd(nc, in_maps, core_ids=list(range(8)))
    if stage < 99:
        return res
    out = np.zeros((4, S, D), np.float32)
    for core in range(8):
        b, half = core // 2, core % 2
        out[b, half * 2048:(half + 1) * 2048] = res.results[core]["out"]
    return out
```

```python
import contextlib
import os
import numpy as np
import ml_dtypes
import concourse.bass as bass
import concourse.mybir as mybir
from concourse.bass_utils import run_bass_kernel_spmd

F32 = mybir.dt.float32
BF16 = mybir.dt.bfloat16
AF = mybir.ActivationFunctionType
ALU = mybir.AluOpType
AX = mybir.AxisListType

D = 1024
S = 4096
H = 16
DH = 64
FF = 3584
NE = 8
NOWN = 17
NCTX = 15
TOWN = NOWN * 128
TCTX = NCTX * 128
NEG = -30000.0

ENGS = ['pe', 'act', 'dve', 'pool', 'sp']
SEM_LIMIT = 30000


class Op:
    __slots__ = ('eng', 'fn', 'deps', 'isdma', 'dsem', 'count', 'needed', 'sig', 'key', 'seq')

    def __init__(self, eng, fn):
        self.eng = eng
        self.fn = fn
        self.deps = []
        self.isdma = False
        self.dsem = None
        self.count = 0
        self.needed = False
        self.sig = None
        self.key = None
        self.seq = 0


class DmaSem:
    def __init__(self, prog, name):
        self.sem = prog.new_sem(name)
        self.count = 0
        self.key = ('dma', name, prog.nsem)
        self.hist = []

    def count_before(self, seq):
        c = 0
        for sq, cn in self.hist:
            if sq < seq:
                c = cn
            else:
                break
        return c


_DSIZE = {}


def _dsize(dt):
    if dt not in _DSIZE:
        _DSIZE[dt] = 2 if dt == BF16 else (4 if dt == F32 else int(np.dtype(dt.np).itemsize))
    return _DSIZE[dt]


def _region(ap):
    t = ap.tensor
    sp = t.space
    if sp == 'SB':
        space = 'sb'
        base = t.manual_sbuf_range[0]
    elif sp == 'PSUM':
        space = 'ps_' + t.name
        base = 0
    else:
        return None
    ds = _dsize(t.dtype)
    shape = list(t.shape)
    rowsize = 1
    for d_ in shape[1:]:
        rowsize *= d_
    apl = [(int(a), int(b)) for a, b in ap.ap]
    off = int(ap.offset)
    pcnt = apl[0][1]
    p0 = off // rowsize
    col0 = off % rowsize
    dims = sorted([(st, c) for st, c in apl[1:] if c > 1], reverse=True)
    starts = [col0]
    k = 0
    while k < len(dims) - 1 and len(starts) * dims[k][1] <= 64:
        st, c = dims[k]
        starts = [s0 + i * st for s0 in starts for i in range(c)]
        k += 1
    span = 1
    for st, c in dims[k:]:
        span += (c - 1) * st
    iv = [(base + s0 * ds, base + (s0 + span) * ds) for s0 in starts]
    lo = min(a for a, _ in iv)
    hi = max(b for _, b in iv)
    return (space, p0, p0 + pcnt, lo, hi, iv)


def _overlap(r1, r2):
    if r1[2] <= r2[1] or r2[2] <= r1[1] or r1[4] <= r2[3] or r2[4] <= r1[3]:
        return False
    i1, i2 = r1[5], r2[5]
    if len(i1) == 1 and len(i2) == 1:
        return True
    for a0, a1 in i1:
        for b0, b1 in i2:
            if a0 < b1 and b0 < a1:
                return True
    return False


def _contains(big, small):
    if big[1] > small[1] or big[2] < small[2]:
        return False
    if len(big[5]) == 1:
        return big[3] <= small[3] and big[4] >= small[4]
    if len(big[5]) == len(small[5]):
        return all(a0 <= b0 and a1 >= b1 for (a0, a1), (b0, b1) in zip(big[5], small[5]))
    return False


class Tracker:
    BK = 2048

    def __init__(self):
        self.buckets = {}
        self.rid = 0

    def _bks(self, r):
        return [(r[0], b) for b in range(r[3] // self.BK, (r[4] - 1) // self.BK + 1)]

    def access(self, op, reads, writes):
        deps = {}
        recs = []
        for ap, isw in [(a, False) for a in reads] + [(a, True) for a in writes]:
            if ap is None or isinstance(ap, (int, float)):
                continue
            r = _region(ap)
            if r is None:
                continue
            seen = set()
            for bk in self._bks(r):
                lst = self.buckets.get(bk)
                if not lst:
                    continue
                for rec in lst:
                    if rec[0] in seen:
                        continue
                    seen.add(rec[0])
                    _, rr, rop, risw = rec
                    if not (isw or risw):
                        continue
                    if rop is op:
                        continue
                    if _overlap(r, rr):
                        deps[id(rop)] = rop
            recs.append((r, isw))
        for r, isw in recs:
            self.rid += 1
            rec = (self.rid, r, op, isw)
            for bk in self._bks(r):
                lst = self.buckets.setdefault(bk, [])
                if isw:
                    lst[:] = [x for x in lst if not _contains(r, x[1])]
                else:
                    lst[:] = [x for x in lst if not ((not x[3]) and x[2].key == op.key and _contains(r, x[1]))]
                lst.append(rec)
        return list(deps.values())


class Prog:
    def __init__(self, nc):
        self.nc = nc
        self.es = contextlib.ExitStack()
        self.ops = {e: [] for e in ENGS}
        self.nsem = 0
        self.trk = Tracker()
        self.nseq = 0

    def new_sem(self, name):
        self.nsem += 1
        return self.es.enter_context(self.nc.semaphore(f"{name}_{self.nsem}"))

    def op(self, eng, fn, waits=(), signal=True, reads=(), writes=()):
        o = Op(eng, fn)
        o.key = eng
        self.nseq += 1
        o.seq = self.nseq
        deps = [w for w in waits if w is not None]
        deps += self.trk.access(o, reads, writes)
        o.deps = deps
        self.ops[eng].append(o)
        return o

    def dma(self, eng, dsem, out, in_, waits=()):
        o = Op(eng, lambda e: e.dma_start(out=out, in_=in_))
        o.isdma = True
        o.dsem = dsem
        o.key = dsem.key
        dsem.count += 16
        o.count = dsem.count
        self.nseq += 1
        o.seq = self.nseq
        dsem.hist.append((o.seq, o.count))
        deps = [w for w in waits if w is not None]
        deps += self.trk.access(o, [in_], [out])
        o.deps = deps
        self.ops[eng].append(o)
        return o

    def wait_only(self, eng, waits):
        o = Op(eng, None)
        o.key = eng
        self.nseq += 1
        o.seq = self.nseq
        o.deps = [w for w in waits if w is not None]
        self.ops[eng].append(o)

    def _reduce_deps(self, o):
        best = {}
        for d in o.deps:
            if d.isdma:
                k = d.dsem.key
                if k not in best or d.count > best[k].count:
                    best[k] = d
            else:
                if d.eng == 'pe' and o.eng == 'pe' and not o.isdma:
                    continue
                k = d.eng
                if k not in best or d.seq > best[k].seq:
                    best[k] = d
        o.deps = list(best.values())

    def emit(self):
        nc = self.nc
        for eng in ENGS:
            for o in self.ops[eng]:
                self._reduce_deps(o)
        for eng in ENGS:
            for o in self.ops[eng]:
                for d in o.deps:
                    if d.isdma:
                        continue
                    if d.eng == 'pe' and o.eng == 'pe' and not o.isdma:
                        continue
                    d.needed = True
        for eng in ENGS:
            cnt = SEM_LIMIT
            gen = 0
            sem = None
            for o in self.ops[eng]:
                if o.isdma or not o.needed:
                    continue
                if cnt >= SEM_LIMIT:
                    gen += 1
                    sem = self.new_sem(f"s_{eng}{gen}")
                    cnt = 0
                cnt += 1
                o.sig = (sem, cnt, (eng, gen))
        nwaits = [0]
        with nc.Block() as block:
            def run(eng_name):
                def body(e):
                    waited = {}
                    for o in self.ops[eng_name]:
                        for d in o.deps:
                            if d.isdma:
                                sem, count, key = d.dsem.sem, max(d.count, d.dsem.count_before(o.seq)), d.dsem.key
                            else:
                                if d.sig is None:
                                    continue
                                sem, count, key = d.sig
                            if waited.get(key, 0) >= count:
                                continue
                            waited[key] = count
                            e.wait_ge(sem, count)
                            nwaits[0] += 1
                        if o.fn is None:
                            continue
                        ins = o.fn(e)
                        if o.isdma:
                            ins.then_inc(o.dsem.sem, 16)
                        elif o.sig is not None:
                            ins.then_inc(o.sig[0], 1)
                return body
            block.tensor(run('pe'))
            block.scalar(run('act'))
            block.vector(run('dve'))
            block.gpsimd(run('pool'))
            block.sync(run('sp'))
        self.stats = {e: len(self.ops[e]) for e in ENGS}
        self.stats['waits'] = nwaits[0]

    def close(self):
        self.es.close()


class Ring:
    def __init__(self, bufs):
        self.bufs = bufs
        self.free = [[] for _ in bufs]
        self.i = 0

    def next(self):
        s = self.i % len(self.bufs)
        self.i += 1
        return s, self.bufs[s], list(self.free[s])

    def release(self, slot, evs):
        self.free[slot] = [e for e in evs if e is not None]


A0 = 0
MODB = 69632
CONST = 94208
B0 = 98304
SB_END = 212736
SB_BASE = 16544


def build_program(stage=99):
    nc = bass.Bass("TRN2", target_bir_lowering=False)
    P = Prog(nc)

    def din(name, shape, dt=F32):
        return nc.dram_tensor(name, list(shape), dt, kind="ExternalInput").ap()

    xo_d = din("xo", [TOWN, D])
    xc_d = din("xc", [TCTX, D])
    cT_d = din("cT", [128, 8])
    kb2_d = din("kb2", [2, S], BF16)
    halo_d = din("halo", [128, 1])
    ident_d = din("ident", [128, 128], BF16)
    identf_d = din("identf", [128, 128])
    trim_d = din("trimask", [128, 128], BF16)
    selk_d = din("selk", [H, 80, 71], BF16)
    selq_d = din("selq", [H, 80, 71], BF16)
    swab_d = din("swab", [128, 4 * 2 * 4 * 128], BF16)
    w_ada_d = din("w_ada", [2, D, 6 * D])
    b_ada_d = din("b_ada", [2, 6 * D])
    gmix_d = din("g_norm_mix", [2, D])
    gffn_d = din("g_norm_ffn", [2, D])
    gfin_d = din("g_final", [1, D])
    fwin_d = din("fox_w_in", [1, D, 3 * D + H])
    fbf_d = din("fox_b_f", [H, 1])
    fwout_d = din("fox_w_out", [1, D, D])
    swin_d = din("swa_w_in", [1, D, D + 512])
    ssink_d = din("swa_sinks", [1, H])
    swout_d = din("swa_w_out", [1, D, D])
    wgu_d = din("ffn_w_gu", [1, D, 2 * FF])
    wdn_d = din("ffn_w_down", [1, FF, D])
    wr_d = din("moe_w_router", [1, D, NE])
    br_d = din("moe_b_router", [1, NE])
    if stage >= 6:
        mgu_d = din("moe_w_gu", [1, NE, D, 2 * FF])
        mdn_d = din("moe_w_down", [1, NE, FF, D])
    out_d = nc.dram_tensor("out", [2048, D], F32, kind="ExternalOutput").ap()
    dbg_d = None
    if stage < 99:
        dbg_d = nc.dram_tensor("dbg", [128, NOWN * D], F32, kind="ExternalOutput").ap()
        dbg2_d = nc.dram_tensor("dbg2", [128, 8 * S], BF16, kind="ExternalOutput").ap()
        dbg3_d = nc.dram_tensor("dbg3", [128, S], F32, kind="ExternalOutput").ap()

    ntens = [0]

    def T(name, shape, dt, off):
        ntens[0] += 1
        assert off % 32 == 0, (name, off)
        sz = int(np.prod(shape[1:])) * (2 if dt == BF16 else 4)
        assert off + sz <= SB_END, (name, off, sz)
        return nc.alloc_sbuf_tensor_at(f"{name}{ntens[0]}", list(shape), dt, offset=off + SB_BASE)

    pb = [nc.alloc_psum_tensor(f"pb{i}", [128, 512], F32) for i in range(8)]
    pbb = [p.bitcast(BF16) for p in pb]

    x_res = T("x_res", [128, NOWN, D], F32, A0)
    modt = [T(f"mod{i}", [128, D], F32, MODB + i * 4096) for i in range(6)]
    SH1, GSC1, GT1, SH2, GSC2, GT2 = range(6)
    ident = T("ident", [128, 128], BF16, CONST)
    trim = T("trim", [128, 128], BF16, CONST + 256)
    cond_rep = T("cond_rep", [128, 8, 128], BF16, CONST + 512)
    condT = T("condT", [128, 8], F32, CONST + 2560)
    cond_s = T("cond_s", [128, 8], F32, CONST + 2592)
    ssq = T("ssq", [128, 64], F32, CONST + 2624)
    rstd = T("rstd", [128, 64], F32, CONST + 2880)
    halo_t = T("halo_t", [128, 1], F32, CONST + 3136)
    bf_col = T("bf_col", [H, 1], F32, CONST + 3168)
    esink = T("esink", [128, H], F32, CONST + 3200)
    brb = T("brb", [128, NE], F32, CONST + 3264)
    small = T("small", [128, 64], F32, CONST + 3296)
    identf = T("identf", [128, 128], F32, CONST + 3584)

    ds_c = DmaSem(P, "dc")
    ds_out = DmaSem(P, "dout")

    def _aps(*xs):
        return [x for x in xs if x is not None and not isinstance(x, (int, float))]

    def act(out, in_, func, waits=(), signal=True, **kw):
        return P.op('act', lambda e: e.activation(out=out, in_=in_, func=func, **kw), waits,
                    reads=_aps(in_, kw.get('bias'), kw.get('scale')), writes=_aps(out, kw.get('accum_out')))

    def mm(out, lhsT, rhs, start, stop, waits=(), signal=False):
        return P.op('pe', lambda e: e.matmul(out, lhsT=lhsT, rhs=rhs, start=start, stop=stop), waits,
                    reads=[lhsT, rhs], writes=[out])

    def tr(out, in_, idn, waits=(), signal=False):
        return P.op('pe', lambda e: e.transpose(out=out, in_=in_, identity=idn), waits, reads=[in_, idn], writes=[out])

    def vtt(out, in0, in1, op, waits=(), signal=True, eng='dve'):
        return P.op(eng, lambda e: e.tensor_tensor(out=out, in0=in0, in1=in1, op=op), waits, reads=[in0, in1], writes=[out])

    def vts(out, in0, s1, s2, op0, op1=None, waits=(), signal=True, eng='dve'):
        if op1 is None:
            return P.op(eng, lambda e: e.tensor_scalar(out=out, in0=in0, scalar1=s1, scalar2=None, op0=op0), waits,
                        reads=_aps(in0, s1), writes=[out])
        return P.op(eng, lambda e: e.tensor_scalar(out=out, in0=in0, scalar1=s1, scalar2=s2, op0=op0, op1=op1), waits,
                    reads=_aps(in0, s1, s2), writes=[out])

    def vstt(out, in0, scalar, in1, op0, op1, waits=(), signal=True, eng='dve'):
        return P.op(eng, lambda e: e.scalar_tensor_tensor(out=out, in0=in0, scalar=scalar, in1=in1, op0=op0, op1=op1), waits,
                    reads=_aps(in0, scalar, in1), writes=[out])

    def vcopy(out, in_, waits=(), signal=True, eng='dve'):
        return P.op(eng, lambda e: e.tensor_copy(out=out, in_=in_), waits, reads=[in_], writes=[out])

    def vrecip(out, in_, waits=()):
        return P.op('dve', lambda e: e.reciprocal(out=out, in_=in_), waits, reads=[in_], writes=[out])

    def memset(ap, val, waits=(), signal=True, eng='pool'):
        return P.op(eng, lambda e: e.memset(ap, val), waits, writes=[ap])

    P.dma('sp', ds_c, ident[:], ident_d)
    P.dma('sp', ds_c, identf[:], identf_d)
    P.dma('sp', ds_c, trim[:], trim_d)
    P.dma('sp', ds_c, halo_t[:], halo_d)
    P.dma('sp', ds_c, bf_col[:], fbf_d)
    P.dma('sp', ds_c, esink[:], ssink_d[0:1, :].partition_broadcast(128))
    P.dma('sp', ds_c, brb[:], br_d[0:1, :].partition_broadcast(128))
    e_const = P.dma('sp', ds_c, condT[:], cT_d)
    e_silu = act(cond_s[:], condT[:], AF.Silu, waits=[e_const])
    e_crep = None
    for k in range(8):
        e_crep = act(cond_rep[:, k, :], ident[:], AF.Identity, waits=[e_silu], scale=0.0, bias=cond_s[:, k:k + 1])
    e_esink = act(esink[:], esink[:], AF.Exp)

    def compute_mods(l, base, prior, groups=range(12)):
        wst = [T("wada", [128, 8, 512], BF16, base + i * 8192) for i in range(2)]
        bbs = [T("bb", [128, 512], F32, base + 16384 + i * 2048) for i in range(2)]
        gb = [T("gb", [128, D], F32, base + 20480 + i * 4096) for i in range(2)]
        tmp = T("modtmp", [128, 512], F32, base + 28672)
        dsw = [DmaSem(P, f"dwa{l}{i}") for i in range(2)]
        dsb = [DmaSem(P, f"dbb{l}{i}") for i in range(2)]
        dsg = DmaSem(P, f"dg{l}")
        P.dma('sp', dsg, gb[0][:], gmix_d[l:l + 1, :].partition_broadcast(128), waits=prior)
        e_g = P.dma('sp', dsg, gb[1][:], gffn_d[l:l + 1, :].partition_broadcast(128), waits=prior)
        wring = Ring(wst)
        bring = Ring(bbs)
        pring = Ring([pb[0], pb[1]])
        wv = w_ada_d[l].rearrange("(k p) n -> p k n", p=128)
        last = None
        evs = []
        for cg in groups:
            ws, wt, wfree = wring.next()
            bs, bt, bfree = bring.next()
            ps, pt, pfree = pring.next()
            e_w = P.dma('pool', dsw[ws], wt[:], wv[:, :, cg * 512:(cg + 1) * 512], waits=wfree + list(prior))
            e_b = P.dma('sp', dsb[bs], bt[:], b_ada_d[l:l + 1, cg * 512:(cg + 1) * 512].partition_broadcast(128),
                        waits=bfree + list(prior))
            e_m = None
            for k in range(8):
                e_m = mm(pt[:], cond_rep[:, k, :], wt[:, k, :], k == 0, k == 7,
                         waits=[e_w, e_crep] + pfree, signal=(k == 7))
            idx, half = cg // 2, cg % 2
            dst = modt[idx][:, half * 512:(half + 1) * 512]
            if idx in (1, 4):
                vtt(tmp[:], pt[:], bt[:], ALU.add, waits=[e_m, e_b, last] + list(prior), signal=False)
                g = gb[0] if idx == 1 else gb[1]
                last = vstt(dst, tmp[:], 1.0, g[:, half * 512:(half + 1) * 512], ALU.add, ALU.mult, waits=[e_g])
            else:
                last = vtt(dst, pt[:], bt[:], ALU.add, waits=[e_m, e_b] + list(prior))
            wring.release(ws, [e_m])
            bring.release(bs, [last])
            pring.release(ps, [last])
            evs.append(last)
        return last

    class NormCtx:
        def __init__(self, base, prior):
            self.t = Ring([T("nt", [128, D], F32, base + i * 4096) for i in range(2)])
            self.hb = Ring([T("nhb", [128, D], BF16, base + 8192 + i * 2048) for i in range(2)])
            self.junk = T("njunk", [128, D], BF16, base + 12288)
            self.pt = Ring([pbb[6], pbb[7]])
            self.prior = list(prior)
            self.n = 0
        SIZE = 14336

    def norm_block(ctx, xin, col, gsc_t, sh_t, hT_dst, waits, h32=None):
        pr = ctx.prior if ctx.n < 2 else []
        ctx.n += 1
        e_sq = act(ctx.junk[:], xin, AF.Square, waits=list(waits) + pr, accum_out=ssq[:, col:col + 1])
        e_sd = act(rstd[:, col:col + 1], ssq[:, col:col + 1], AF.Sqrt, waits=[e_sq], scale=1.0 / D, bias=small[:, 0:1])
        e_r = vrecip(rstd[:, col:col + 1], rstd[:, col:col + 1], waits=[e_sd] + pr)
        ts, tt, tfree = ctx.t.next()
        hs, hb, hfree = ctx.hb.next()
        e_t = vstt(tt[:], xin, rstd[:, col:col + 1], gsc_t[:], ALU.mult, ALU.mult, waits=list(waits) + tfree + pr + [e_r])
        if h32 is not None:
            vtt(h32[:], tt[:], sh_t[:], ALU.add)
            e_h = vcopy(hb[:], h32[:], waits=hfree)
        else:
            e_h = vtt(hb[:], tt[:], sh_t[:], ALU.add, waits=hfree)
        ctx.t.release(ts, [e_h])
        ps, pt, pfree = ctx.pt.next()
        e_tr = None
        for k in range(8):
            e_tr = tr(pt[:, k * 128:(k + 1) * 128], hb[:, k * 128:(k + 1) * 128], ident[:],
                      waits=[e_h] + pfree + pr, signal=(k == 7))
        ctx.hb.release(hs, [e_tr])

        def fin():
            e_c = act(hT_dst, pt[:, :].rearrange("p (k t) -> p k t", k=8), AF.Copy, waits=[e_tr])
            ctx.pt.release(ps, [e_c])
            return e_c
        norm_flush(ctx)
        ctx.pending = fin
        return None, e_t, e_sq

    def norm_flush(ctx):
        e = None
        if getattr(ctx, "pending", None) is not None:
            e = ctx.pending()
            ctx.pending = None
        return e

    e_eps = memset(small[:, 0:1], 1e-6, eng='dve')
    e_one = memset(small[:, 1:2], 1.0, eng='dve')

    compute_mods(0, B0, [], range(0, 4))

    hT = T("hT", [128, 8, S], BF16, 0)
    PT = [T("PT", [128, 384], BF16, 65536 + i * 768) for i in range(4)]
    selk_t = [T("selk", [80, 71], BF16, 68608 + i * 160) for i in range(2)]
    selq_t = [T("selq", [80, 71], BF16, 68928 + i * 160) for i in range(2)]
    oT = T("oT", [128, 8, TOWN], BF16, B0)
    o_pair = [T("opair", [128, NOWN, 128], BF16, B0 + 34816 + i * 4352) for i in range(2)]
    V_grp = T("Vgrp", [128, 32, 4, 65], BF16, B0 + 43520)
    Fsrc = T("Fsrc", [80, S], BF16, B0 + 60160)
    KT = [T("KT", [71, S], BF16, B0 + 68352 + i * 8192) for i in range(2)]
    QT = [T("QT", [71, TOWN], BF16, B0 + 84736 + i * 4352) for i in range(2)]
    Wqk = [T("Wqk", [128, 8, 142], BF16, B0 + 93440 + i * 2272) for i in range(2)]
    Wv = T("Wv", [128, 8, 256], BF16, B0 + 97984)
    Wf = T("Wf", [128, 8, 16], BF16, B0 + 102080)
    NB = B0 + 68352
    xin_bufs = [T("xin", [128, D], F32, NB + 14336 + i * 4096) for i in range(2)]

    nctx = NormCtx(NB, [])
    xring = Ring(xin_bufs)
    ds_x = [DmaSem(P, f"dx{i}") for i in range(2)]
    e_hT = None
    for i in range(32):
        src = xc_d[i * 128:(i + 1) * 128, :] if i < NCTX else xo_d[(i - NCTX) * 128:(i - NCTX + 1) * 128, :]
        xs, xb, xfree = xring.next()
        e_ld = P.dma('sp', ds_x[xs], xb[:], src, waits=xfree)
        e_c, e_t, e_sq = norm_block(nctx, xb[:], i, modt[GSC1], modt[SH1], hT[:, :, i * 128:(i + 1) * 128],
                                    [e_ld, e_eps])
        xring.release(xs, [e_t, e_sq])
    e_hT = norm_flush(nctx)
    compute_mods(0, B0, [], range(4, 12))

    if stage == 1:
        e1 = P.dma('sp', ds_out, dbg2_d, hT[:, :, :].rearrange("p k t -> p (k t)"), waits=[e_hT])
        P.wait_only('sp', [e1])
        P.emit()
        P.close()
        return nc


    X1 = T("X1", [H, S], F32, B0)
    X2 = T("X2", [H, 2048], F32, B0 + 16384)
    X3 = T("X3", [H, 2048], F32, B0 + 24576)
    thi = T("thi", [H, 2048], BF16, B0 + 32768)
    tmid = T("tmid", [H, 2048], BF16, B0 + 36864)
    tlo = T("tlo", [H, 2048], BF16, B0 + 40960)
    wvv = fwin_d[0].rearrange("(k p) n -> p k n", p=128)
    ds_wf = DmaSem(P, "dwf")
    e_wf = P.dma('pool', ds_wf, Wf[:], wvv[:, :, 3 * D:3 * D + H])
    e_fz = memset(Fsrc[:], 0.0, eng='pool')
    ds_kb = DmaSem(P, "dkb")
    e_kb = P.dma('sp', ds_kb, Fsrc[16:18, :], kb2_d, waits=[e_fz])
    ds_f = DmaSem(P, "dfs")
    projring = Ring([pb[6], pb[7]])
    projringb = {id(pb[6]): pbb[6], id(pb[7]): pbb[7]}
    e_prev = None
    e_fdma = []
    for c in range(2):
        sl = slice(c * 2048, (c + 1) * 2048)
        for t4 in range(4):
            tg = c * 4 + t4
            ps_, pt, pfree = projring.next()
            e_m = None
            for k in range(8):
                e_m = mm(pt[0:H, :], Wf[:, k, :], hT[:, k, tg * 512:(tg + 1) * 512], k == 0, k == 7,
                         waits=[e_wf, e_hT] + pfree, signal=(k == 7))
            e_z = act(X1[:, tg * 512:(tg + 1) * 512], pt[0:H, :], AF.Identity, waits=[e_m] + e_fdma, bias=bf_col[:, 0:1])
            projring.release(ps_, [e_z])
        e = act(X2[:], X1[:, sl], AF.Abs, waits=[e_z] + e_fdma)
        e = act(X2[:], X2[:], AF.Exp, waits=[e], scale=-1.0)
        e = act(X2[:], X2[:], AF.Ln, waits=[e, e_one], bias=small[0:H, 1:2])
        e = vstt(X3[:], X1[:, sl], 0.0, X2[:], ALU.min, ALU.subtract, waits=[e] + e_fdma)
        e = memset(X2[:], 1.0, waits=[e], eng='dve')
        init = 0.0 if c == 0 else X1[:, c * 2048 - 1:c * 2048]
        e = P.op('dve', lambda en, sl=sl, init=init: en.tensor_tensor_scan(
            out=X1[:, sl], data0=X2[:], data1=X3[:], initial=init, op0=ALU.mult, op1=ALU.add), waits=[e, e_prev],
            reads=_aps(X2[:], X3[:], init), writes=[X1[:, sl]])
        e_prev = e
        e1 = act(thi[:], X1[:, sl], AF.Identity, waits=[e] + e_fdma, scale=8.0)
        e = vstt(X3[:], X1[:, sl], 8.0, thi[:], ALU.mult, ALU.subtract, waits=[e1])
        e2 = act(tmid[:], X3[:], AF.Copy, waits=[e] + e_fdma)
        e = vtt(X2[:], X3[:], tmid[:], ALU.subtract, waits=[e2])
        e3 = act(tlo[:], X2[:], AF.Copy, waits=[e] + e_fdma)
        e_fdma = [P.dma('sp', ds_f, Fsrc[0:16, sl], thi[:], waits=[e1, e_fz]),
                  P.dma('sp', ds_f, Fsrc[32:48, sl], tmid[:], waits=[e2]),
                  P.dma('sp', ds_f, Fsrc[64:80, sl], tlo[:], waits=[e3])]
    e_F = [e_fdma[-1], e_kb]

    if stage == 15:
        e1 = P.dma('sp', ds_out, dbg3_d[0:H, :], X1[:], waits=e_F)
        e2 = P.dma('sp', ds_out, dbg2_d[0:80, 0:S], Fsrc[:], waits=e_F)
        P.wait_only('sp', [e2])
        P.emit()
        P.close()
        return nc

    ds_qk = [DmaSem(P, f"dqk{i}") for i in range(2)]
    ds_sel = [DmaSem(P, f"dsel{i}") for i in range(2)]
    ds_wv = DmaSem(P, "dwv")
    sring = Ring([pb[0], pb[1], pb[5]])
    ptring = Ring(PT)
    Obank = [pb[2], pb[3], pb[4]]
    Ofree = [[], [], []]
    SBS = [(0, 2), (2, 5), (5, 8), (8, 11), (11, 14), (14, 17)]
    QTG = [(0, 512), (512, 512), (1024, 512), (1536, 512), (2048, 128)]

    def head_tiles():
        tl = []
        for (i0, i1) in SBS:
            nq = i1 - i0
            for kb in range(0, NCTX + i1):
                j0 = max(0, kb - NCTX - i0)
                tl.append((i0, nq, kb, j0, kb >= NCTX + i0))
        return tl
    TILES = head_tiles()

    state = {"proj_done": {}, "kq_ready": {}, "wload": {}, "v_ready": None, "v_last_read": [e_fdma[-1]],
             "last_pv": None}

    def load_head_w(h):
        slot = h % 2
        w = state["proj_done"].get(h - 2, [])
        if h < 2:
            memset(Wqk[slot][:, :, 64:71], 0.0, eng='pool')
            memset(Wqk[slot][:, :, 135:142], 0.0, eng='pool')
        P.dma('pool', ds_qk[slot], Wqk[slot][:, :, 0:64], wvv[:, :, h * 64:(h + 1) * 64], waits=w)
        e1 = P.dma('pool', ds_qk[slot], Wqk[slot][:, :, 71:135], wvv[:, :, D + h * 64:D + (h + 1) * 64], waits=w)
        P.dma('sp', ds_sel[slot], selk_t[slot][:], selk_d[h], waits=w)
        e2 = P.dma('sp', ds_sel[slot], selq_t[slot][:], selq_d[h], waits=w)
        state["wload"][h] = [e1, e2]

    def proj_units(h):
        slot = h % 2
        units = []
        evs = []
        state["kq_ready"][h] = evs

        def unit(kind, c0, n, dst):
            def f():
                ps_, pt, pfree = projring.next()
                wl = state["wload"][h] + e_F + pfree
                if kind == 'k':
                    mm(pt[0:71, 0:n], selk_t[slot][:, :], Fsrc[:, c0:c0 + n], True, False, waits=wl)
                    wc = slice(71, 142)
                else:
                    mm(pt[0:71, 0:n], selq_t[slot][:, :], Fsrc[:, c0:c0 + n], True, False, waits=wl)
                    wc = slice(0, 71)
                e_m = None
                for k in range(8):
                    e_m = mm(pt[0:71, 0:n], Wqk[slot][:, k, wc], hT[:, k, c0:c0 + n], False, k == 7, signal=(k == 7))
                e_c = vcopy(dst, pt[0:71, 0:n], waits=[e_m])
                projring.release(ps_, [e_c])
                evs.append(e_c)
                state["proj_done"][h] = [e_m]
            return f
        for tg in range(8):
            units.append(unit('k', tg * 512, 512, KT[slot][:, tg * 512:(tg + 1) * 512]))
        for (st, n) in QTG:
            units.append(unit('q', TCTX + st, n, QT[slot][:, st:st + n]))
        return units

    def compute_V(g):
        w = state["v_last_read"]
        e_w = P.dma('pool', ds_wv, Wv[:], wvv[:, :, 2 * D + g * 256:2 * D + (g + 1) * 256], waits=w)
        evs = []
        if g == 0:
            evs.append(memset(V_grp[:, :, :, 64:65], 1.0, waits=w, eng='pool'))
        for tb2 in range(16):
            ps_, pt, pfree = projring.next()
            e_m = None
            for j in range(2):
                tb = tb2 * 2 + j
                for k in range(8):
                    e_m = mm(pt[:, j * 256:(j + 1) * 256], hT[:, k, tb * 128:(tb + 1) * 128], Wv[:, k, :], k == 0, k == 7,
                             waits=[e_w] + pfree, signal=(j == 1 and k == 7))
            src = pt[:, :].rearrange("p (j h d) -> p j h d", j=2, h=4)
            dst = V_grp[:, tb2 * 2:tb2 * 2 + 2, :, 0:64]
            e_c = vcopy(dst, src, waits=[e_m] + w)
            projring.release(ps_, [e_c])
            evs.append(e_c)
        state["v_ready"] = evs
        state["v_last_read"] = [e_m]

    pend = []

    def emit_S(h, tile):
        i0, nq, kb, j0, diag = tile
        slot = h % 2
        ss, sb_, sfree = sring.next()
        c0, c1 = j0 * 128, nq * 128
        e_s = mm(sb_[:, c0:c1], KT[slot][:, kb * 128:(kb + 1) * 128], QT[slot][:, i0 * 128 + c0:i0 * 128 + c1],
                 True, not diag, waits=state["kq_ready"][h] + sfree, signal=not diag)
        if diag:
            e_s = mm(sb_[:, c0:c0 + 128], ident[:], trim[:], False, True, signal=True)
        return (h, tile, ss, sb_, e_s)

    def emit_EXP_PV(rec):
        h, tile, ss, sb_, e_s = rec
        i0, nq, kb, j0, diag = tile
        c0, c1 = j0 * 128, nq * 128
        ps_, ptb, pfree = ptring.next()
        e_x = act(ptb[:, c0:c1], sb_[:, c0:c1], AF.Exp, waits=[e_s] + pfree, scale=0.125)
        sring.release(ss, [e_x])
        hh = h % 4
        e_pv = None
        for j in range(j0, nq):
            first = (kb == 0)
            last = (kb == NCTX + i0 + j)
            wl = [e_x] + state["v_ready"]
            if first:
                wl = wl + Ofree[j]
            is_last_of_tile = (j == nq - 1)
            e_pv = mm(Obank[j][:, 0:65], ptb[:, j * 128:(j + 1) * 128], V_grp[:, kb, hh, :], first, last,
                      waits=wl, signal=(last or is_last_of_tile))
            if last:
                col = 8 + j
                vts(small[:, col:col + 1], Obank[j][:, 64:65], 1e-30, None, ALU.max, waits=[e_pv])
                e_r = vrecip(small[:, col:col + 1], small[:, col:col + 1])
                pslot = (h // 2) % 2
                e_n = vts(o_pair[pslot][:, i0 + j, hh % 2 * 64:hh % 2 * 64 + 64], Obank[j][:, 0:64],
                          small[:, col:col + 1], None, ALU.mult, waits=[e_r] + state.get("opair_free%d" % pslot, []))
                Ofree[j] = [e_n]
                state["last_norm"] = e_n
        ptring.release(ps_, [e_pv])
        state["last_pv"] = e_pv

    def emit_opair_T(h):
        pair = h // 2
        pslot = pair % 2
        e_last = None
        for i0_ in range(0, NOWN, 8):
            n = min(8, NOWN - i0_)
            ps_, pt, pfree = projring.next()
            ptv = projringb[id(pt)]
            e_t = None
            for i in range(n):
                e_t = tr(ptv[:, i * 128:(i + 1) * 128], o_pair[pslot][:, i0_ + i, :], ident[:],
                         waits=[state["last_norm"]] + pfree, signal=(i == n - 1))
            e_c = vcopy(oT[:, pair, i0_ * 128:(i0_ + n) * 128], ptv[:, 0:n * 128], waits=[e_t])
            projring.release(ps_, [e_c])
            e_last = e_t
        state["opair_free%d" % pslot] = [e_last]
        state["oT_last"] = e_c

    load_head_w(0)
    compute_V(0)
    for u in proj_units(0):
        u()
    for h in range(H):
        nxt = []
        if h + 1 < H:
            load_head_w(h + 1)
            nxt = proj_units(h + 1)
        recs = [emit_S(h, TILES[0]), emit_S(h, TILES[1])]
        nt = len(TILES)
        every = max(1, nt // (len(nxt) + 1)) if nxt else nt + 1
        for t in range(nt):
            if t + 2 < nt:
                recs.append(emit_S(h, TILES[t + 2]))
            emit_EXP_PV(recs[t])
            if nxt and (t % every == every - 1):
                nxt.pop(0)()
        while nxt:
            nxt.pop(0)()
        if h % 2 == 1:
            emit_opair_T(h)
        if h + 1 < H and (h + 1) % 4 == 0:
            state["v_last_read"] = [state["last_pv"]]
            compute_V((h + 1) // 4)

    if stage == 2:
        e1 = P.dma('sp', ds_out, dbg2_d[:, 0:8 * TOWN], oT[:, :, :].rearrange("p k t -> p (k t)"), waits=[state["oT_last"]])
        P.wait_only('sp', [e1])
        P.emit()
        P.close()
        return nc


    def finish_dbg():
        e1 = P.dma('sp', ds_out, dbg_d, x_res[:, :, :].rearrange("p i d -> p (i d)"))
        P.wait_only('sp', [e1])
        P.emit()
        P.close()
        return nc

    def out_proj(w_src, name):
        Wo = T("Wo" + name, [128, 8, D], BF16, B0 + 93440)
        ytmp = [T("ytmp" + name, [128, 512], F32, B0 + 109824 + i * 2048) for i in range(2)]
        ds_wo = DmaSem(P, "dwo" + name)
        wsrc = w_src.rearrange("(k p) n -> p k n", p=128)
        for k2 in range(2):
            P.dma('pool', ds_wo, Wo[:, :, k2 * 512:(k2 + 1) * 512], wsrc[:, :, k2 * 512:(k2 + 1) * 512])
        return Wo, ytmp

    Wo, ytmp = out_proj(fwout_d[0], "f")
    ds_xr = DmaSem(P, "dxr")
    for i in range(NOWN):
        P.dma('sp', ds_xr, x_res[:, i, :], xo_d[i * 128:(i + 1) * 128, :])
    n_y = 0
    for i in range(NOWN):
        for half in range(2):
            bank = pb[n_y % 2]
            yt = ytmp[n_y % 2]
            n_y += 1
            hs = slice(half * 512, (half + 1) * 512)
            for k in range(8):
                mm(bank[:, :], oT[:, k, i * 128:(i + 1) * 128], Wo[:, k, hs], k == 0, k == 7)
            vtt(yt[:], bank[:], modt[GT1][:, hs], ALU.mult)
            vtt(x_res[:, i, hs], x_res[:, i, hs], yt[:], ALU.add, eng='pool')

    if stage == 3:
        return finish_dbg()

    FB = B0
    hT2 = T("hT2", [128, 8, TOWN], BF16, FB)
    actT = [T("actT", [128, 4, 512], BF16, FB + 34816 + i * 4096) for i in range(2)]
    wgu_t = [T("wgu", [128, 8, 1024], BF16, FB + 43008 + i * 16384) for i in range(2)]
    wd_t = [T("wd", [128, 4, D], BF16, FB + 75776 + i * 8192) for i in range(2)]
    sg_t = [T("sg", [128, 512], F32, FB + 92160 + i * 2048) for i in range(2)]
    NB2 = FB + 96256
    gates = T("gates", [128, NOWN * NE], F32, FB + 110592)
    ds_gu = [DmaSem(P, f"dgu{i}") for i in range(2)]
    ds_wd = [DmaSem(P, f"dwd{i}") for i in range(2)]
    ffn_state = {"stage": 0, "g": 0, "u": 0, "y": 0, "a": 0, "s": 0}
    Gb = [pb[0], pb[1]]
    Ub = [pb[2], pb[3]]
    Yb = [pb[4], pb[5]]

    def ffn_pass(gsrc, usrc, dsrc, blk0, blk1, gt_tile, expert=None):
        groups = [(b, min(b + 4, blk1)) for b in range(blk0, blk1, 4)]
        st = ffn_state

        def emit_down(aslot, b0, b1, slot):
            for b in range(b0, b1):
                for half in range(2):
                    Y = Yb[st["y"] % 2]
                    st["y"] += 1
                    hs = slice(half * 512, (half + 1) * 512)
                    for fc in range(4):
                        mm(Y[:, :], actT[aslot][:, fc, (b - b0) * 128:(b - b0 + 1) * 128], wd_t[slot][:, fc, hs], fc == 0, fc == 3)
                    if expert is None:
                        vtt(x_res[:, b, hs], Y[:, :], x_res[:, b, hs], ALU.add)
                    else:
                        vstt(x_res[:, b, hs], Y[:, :], gates[:, b * NE + expert:b * NE + expert + 1], x_res[:, b, hs],
                             ALU.mult, ALU.add)

        for fg in range(7):
            slot = st["stage"] % 2
            st["stage"] += 1
            fs = slice(fg * 512, (fg + 1) * 512)
            P.dma('pool', ds_gu[slot], wgu_t[slot][:, :, 0:512], gsrc[:, :, fs])
            P.dma('pool', ds_gu[slot], wgu_t[slot][:, :, 512:1024], usrc[:, :, fs])
            P.dma('pool', ds_wd[slot], wd_t[slot][:, :, :], dsrc[fg * 512:(fg + 1) * 512, :].rearrange("(c p) n -> p c n", p=128))
            for c in range(4):
                vtt(wd_t[slot][:, c, :], wd_t[slot][:, c, :], gt_tile[:], ALU.mult, eng='pool')
            pending = None
            for (b0, b1) in groups:
                ntok = (b1 - b0) * 128
                t0 = b0 * 128
                aslot = st["a"] % 2
                st["a"] += 1
                for fc in range(4):
                    G = Gb[st["g"] % 2]
                    U = Ub[st["g"] % 2]
                    st["g"] += 1
                    sg = sg_t[st["s"] % 2]
                    st["s"] += 1
                    for k in range(8):
                        mm(G[:, 0:ntok], wgu_t[slot][:, k, fc * 128:(fc + 1) * 128], hT2[:, k, t0:t0 + ntok], k == 0, k == 7)
                    for k in range(8):
                        mm(U[:, 0:ntok], wgu_t[slot][:, k, 512 + fc * 128:512 + (fc + 1) * 128], hT2[:, k, t0:t0 + ntok],
                           k == 0, k == 7)
                    act(sg[:, 0:ntok], G[:, 0:ntok], AF.Silu)
                    vtt(actT[aslot][:, fc, 0:ntok], sg[:, 0:ntok], U[:, 0:ntok], ALU.mult)
                if pending is not None:
                    emit_down(*pending)
                pending = (aslot, b0, b1, slot)
            emit_down(*pending)

    nctx2 = NormCtx(NB2, [])
    for i in range(NOWN):
        norm_block(nctx2, x_res[:, i, :], 32 + i, modt[GSC2], modt[SH2], hT2[:, :, i * 128:(i + 1) * 128], [])
    norm_flush(nctx2)
    gu0 = wgu_d[0].rearrange("(k p) n -> p k n", p=128)
    ffn_pass(gu0[:, :, 0:FF], gu0[:, :, FF:2 * FF], wdn_d[0], 0, NOWN, modt[GT2])

    if stage == 4:
        return finish_dbg()

    e_mod1 = compute_mods(1, FB + 43008, [])

    nctx3 = NormCtx(NB2, [])
    for i in range(NOWN):
        norm_block(nctx3, x_res[:, i, :], i, modt[GSC1], modt[SH1], hT2[:, :, i * 128:(i + 1) * 128], [])
    norm_flush(nctx3)
    KT2 = T("KT2", [128, 4, TOWN], BF16, FB + 34816)
    V2 = T("V2", [128, NOWN, 4, 65], BF16, FB + 52224)
    QT2 = T("QT2", [128, 8, 2048], BF16, FB + 61088)
    swab_t = T("swab", [128, 4 * 2 * 4 * 128], BF16, FB + 93856)
    wst = T("wst", [128, 8, 512], BF16, FB + 93856)
    PT2 = [T("PT2", [128, 512], BF16, FB + 102048 + i * 1024) for i in range(4)]
    otok = [T("otok", [128, D], BF16, FB + 106144 + i * 2048) for i in range(2)]
    oTb = [T("oTb", [128, 8, 128], BF16, FB + 110240 + i * 2048) for i in range(2)]
    Wo2 = T("Wo2", [128, 8, D], BF16, FB)
    ytmp2 = [T("ytmp2", [128, 512], F32, FB + 16384 + i * 2048) for i in range(2)]
    swv = swin_d[0].rearrange("(k p) n -> p k n", p=128)
    ds_ws = DmaSem(P, "dws")
    for g in range(4):
        for dup in range(2):
            P.dma('pool', ds_ws, wst[:, :, g * 128 + dup * 64:g * 128 + dup * 64 + 64], swv[:, :, D + g * 64:D + (g + 1) * 64])
    npj = 0
    for (st_, n) in [(0, 512), (512, 512), (1024, 512), (1536, 512), (2048, 128)]:
        for g in range(4):
            pt = pb[6 + npj % 2]
            npj += 1
            for k in range(8):
                mm(pt[:, 0:n], wst[:, k, g * 128:(g + 1) * 128], hT2[:, k, st_:st_ + n], k == 0, k == 7)
            if npj % 2 == 0:
                act(KT2[:, g, st_:st_ + n], pt[:, 0:n], AF.Copy)
            else:
                vcopy(KT2[:, g, st_:st_ + n], pt[:, 0:n])
    memset(V2[:, :, :, 64:65], 1.0, eng='pool')
    P.dma('pool', ds_ws, wst[:, :, 0:256], swv[:, :, D + 256:D + 512])
    for i in range(NOWN):
        pt = pb[6 + npj % 2]
        npj += 1
        for k in range(8):
            mm(pt[:, 0:256], hT2[:, k, i * 128:(i + 1) * 128], wst[:, k, 0:256], k == 0, k == 7)
        src = pt[:, 0:256].rearrange("p (g d) -> p g d", g=4)
        if npj % 2 == 0:
            act(V2[:, i, :, 0:64], src, AF.Copy)
        else:
            vcopy(V2[:, i, :, 0:64], src)
    for qh in range(2):
        P.dma('pool', ds_ws, wst[:, :, :], swv[:, :, qh * 512:(qh + 1) * 512])
        for pr_ in range(4):
            pair = qh * 4 + pr_
            for tg in range(4):
                pt = pb[6 + npj % 2]
                npj += 1
                for k in range(8):
                    mm(pt[:, :], wst[:, k, pr_ * 128:(pr_ + 1) * 128], hT2[:, k, 128 + tg * 512:128 + (tg + 1) * 512], k == 0, k == 7)
                act(QT2[:, pair, tg * 512:(tg + 1) * 512], pt[:, :], AF.Identity, scale=0.125)
    ds_w2 = DmaSem(P, "dw2")
    wo2src = swout_d[0].rearrange("(k p) n -> p k n", p=128)
    for k2 in range(2):
        P.dma('pool', ds_w2, Wo2[:, :, k2 * 512:(k2 + 1) * 512], wo2src[:, :, k2 * 512:(k2 + 1) * 512])
    ds_swab = DmaSem(P, "dswab")
    P.dma('sp', ds_swab, swab_t[:], swab_d)
    n_pt = 0
    n_y = 0
    swa_ofree = [[], []]
    for i in range(1, NOWN):
        ot = otok[i % 2]
        for g in range(4):
            pts = []
            npair = (i * 4 + g) % 2
            for p_ in range(2):
                Sb = pb[2 * npair + p_]
                kblk = i - 1 + p_
                for hd in range(4):
                    h = 4 * g + hd
                    pair, par = h // 2, h % 2
                    b0_ = (g * 2 + p_) * 512 + hd * 128
                    mm(Sb[:, hd * 128:(hd + 1) * 128], ident[:], swab_t[:, b0_:b0_ + 128], True, False)
                    mm(Sb[:, hd * 128:(hd + 1) * 128], KT2[par * 64:(par + 1) * 64, g, kblk * 128:(kblk + 1) * 128],
                       QT2[par * 64:(par + 1) * 64, pair, (i - 1) * 128:i * 128], False, True)
                ptb = PT2[n_pt % 4]
                n_pt += 1
                if i == 1 and p_ == 0:
                    act(ptb[:, :], Sb[:, :], AF.Exp, bias=halo_t[:, 0:1])
                else:
                    act(ptb[:, :], Sb[:, :], AF.Exp)
                pts.append(ptb)
            Ob = pb[4 + npair]
            e_pv = None
            for hd in range(4):
                oc = hd * 128
                for p_ in range(2):
                    e_pv = mm(Ob[:, oc:oc + 65], pts[p_][:, hd * 128:(hd + 1) * 128], V2[:, i - 1 + p_, g, :], p_ == 0, p_ == 1,
                              waits=swa_ofree[npair])
            Obv = Ob[:, :].rearrange("p (h c) -> p h c", c=128)
            c0_ = 16 + 4 * npair
            den = small[:, c0_:c0_ + 4]
            vtt(den, Obv[:, :, 64], esink[:, 4 * g:4 * g + 4], ALU.add, waits=[e_pv])
            vrecip(den, den)
            denb = den.rearrange("p (a o) -> p a o", o=1).broadcast_to([128, 4, 64])
            e_n = vtt(ot[:, g * 256:(g + 1) * 256].rearrange("p (h d) -> p h d", h=4), Obv[:, :, 0:64], denb, ALU.mult)
            swa_ofree[npair] = [e_n]
        ob = oTb[i % 2]
        ptv = pbb[6]
        for k in range(8):
            tr(ptv[:, k * 128:(k + 1) * 128], ot[:, k * 128:(k + 1) * 128], ident[:])
        act(ob[:, :, :], ptv[:, :].rearrange("p (k t) -> p k t", k=8), AF.Copy)
        for half in range(2):
            yt = ytmp2[n_y % 2]
            n_y += 1
            hs = slice(half * 512, (half + 1) * 512)
            for k in range(8):
                mm(pb[7][:, :], ob[:, k, :], Wo2[:, k, hs], k == 0, k == 7)
            vtt(yt[:], pb[7][:, :], modt[GT1][:, hs], ALU.mult)
            vtt(x_res[:, i, hs], x_res[:, i, hs], yt[:], ALU.add, eng='pool')

    if stage == 5:
        return finish_dbg()

    h32s = [T("h32", [128, D], F32, FB + 43008 + i * 8448) for i in range(2)]
    hT32s = [T("hT32", [128, 8, 128], F32, FB + 43008 + 4096 + i * 8448) for i in range(2)]
    wr32 = T("wr32", [128, 8, NE], F32, FB + 43008 + 16896)
    RB = FB + 43008 + 17152
    LA = T("LA", [128, 16, NE], F32, RB)
    EQ1 = T("EQ1", [128, 16, NE], F32, RB + 512)
    L2 = T("L2", [128, 16, NE], F32, RB + 1024)
    EQ2 = T("EQ2", [128, 16, NE], F32, RB + 1536)
    M1 = T("M1", [128, 16], F32, RB + 2048)
    M2 = T("M2", [128, 16], F32, RB + 2112)
    Dm = T("Dm", [128, 16], F32, RB + 2176)
    Ed = T("Ed", [128, 16], F32, RB + 2240)
    W1 = T("W1", [128, 16], F32, RB + 2304)
    W2 = T("W2", [128, 16], F32, RB + 2368)
    ds_wr = DmaSem(P, "dwr")
    P.dma('sp', ds_wr, wr32[:], wr_d[0].rearrange("(k p) n -> p k n", p=128))
    nctx4 = NormCtx(NB2, [])
    for i in range(1, NOWN):
        h32, hT32 = h32s[i % 2], hT32s[i % 2]
        norm_block(nctx4, x_res[:, i, :], 32 + i, modt[GSC2], modt[SH2], hT2[:, :, i * 128:(i + 1) * 128], [], h32=h32)
        for half in range(2):
            pt = pb[half + 2 * (i % 2)]
            for k4 in range(4):
                k = half * 4 + k4
                tr(pt[:, k4 * 128:(k4 + 1) * 128], h32[:, k * 128:(k + 1) * 128], identf[:])
            vcopy(hT32[:, half * 4:half * 4 + 4, :], pt[:, :].rearrange("p (k t) -> p k t", k=4))
        lgb = pb[4 + i % 2]
        for k in range(8):
            mm(lgb[:, 0:NE], hT32[:, k, :], wr32[:, k, :], k == 0, k == 7)
        vtt(LA[:, i - 1, :], lgb[:, 0:NE], brb[:], ALU.add)
    norm_flush(nctx4)

    def bc(v):
        return v.rearrange("p (a o) -> p a o", o=1).broadcast_to([128, 16, NE])
    P.op('dve', lambda e: e.tensor_reduce(out=M1[:], in_=LA[:], axis=AX.X, op=ALU.max), reads=[LA[:]], writes=[M1[:]])
    vtt(EQ1[:], LA[:], bc(M1[:]), ALU.is_equal)
    vstt(L2[:], EQ1[:], -1e30, LA[:], ALU.mult, ALU.add)
    P.op('dve', lambda e: e.tensor_reduce(out=M2[:], in_=L2[:], axis=AX.X, op=ALU.max), reads=[L2[:]], writes=[M2[:]])
    vtt(EQ2[:], L2[:], bc(M2[:]), ALU.is_equal)
    vtt(Dm[:], M2[:], M1[:], ALU.subtract)
    act(Ed[:], Dm[:], AF.Exp)
    vts(W1[:], Ed[:], 1.0, None, ALU.add)
    vrecip(W1[:], W1[:])
    vtt(W2[:], Ed[:], W1[:], ALU.mult)
    vtt(EQ1[:], EQ1[:], bc(W1[:]), ALU.mult)
    vtt(EQ2[:], EQ2[:], bc(W2[:]), ALU.mult)
    vtt(gates[:, NE:NOWN * NE].rearrange("p (a b) -> p a b", b=NE), EQ1[:], EQ2[:], ALU.add)

    for e_ in range(NE):
        mg = mgu_d[0, e_].rearrange("(k p) n -> p k n", p=128)
        ffn_pass(mg[:, :, 0:FF], mg[:, :, FF:2 * FF], mdn_d[0, e_], 1, NOWN, modt[GT2], expert=e_)

    if stage == 6:
        return finish_dbg()

    gfin = modt[SH1]
    ds_gf = DmaSem(P, "dgf")
    P.dma('sp', ds_gf, gfin[:], gfin_d[0:1, :].partition_broadcast(128))
    fo = [T("fo", [128, D], F32, NB2 + i * 4096) for i in range(2)]
    fjunk = T("fjunk", [128, D], BF16, NB2 + 8192)
    last = []
    for i in range(1, NOWN):
        col = i
        act(fjunk[:], x_res[:, i, :], AF.Square, accum_out=ssq[:, col:col + 1])
        act(rstd[:, col:col + 1], ssq[:, col:col + 1], AF.Sqrt, scale=1.0 / D, bias=small[:, 0:1])
        vrecip(rstd[:, col:col + 1], rstd[:, col:col + 1])
        f = fo[i % 2]
        vstt(f[:], x_res[:, i, :], rstd[:, col:col + 1], gfin[:], ALU.mult, ALU.mult)
        last.append(P.dma('sp', ds_out, out_d[(i - 1) * 128:i * 128, :], f[:]))
    P.wait_only('sp', last)
    P.emit()
    P.close()
    return nc


def t5_band_buckets():
    CHUNK, BAND, WC = 64, 192, 2
    q = np.arange(CHUNK)[:, None]
    k = np.arange(BAND)[None, :] - WC * CHUNK
    rel = k - q
    nb = 16
    max_exact = nb // 2
    ret = (rel > 0).astype(np.int32) * nb
    n = np.abs(rel)
    large = max_exact + (np.log(np.maximum(n, 1) / max_exact)
                         / np.log(128 / max_exact) * (nb - max_exact)).astype(np.int32)
    large = np.minimum(large, nb - 1)
    return (ret + np.where(n < max_exact, n, large)).astype(np.int32)


def make_inputs(inputs, stage=99):
    bf = ml_dtypes.bfloat16
    x = np.asarray(inputs["x"], dtype=np.float32)
    c = np.asarray(inputs["c"], dtype=np.float32)
    ident = np.eye(128, dtype=np.float32)
    kk = np.arange(128)[:, None]
    qq = np.arange(128)[None, :]
    trimask = np.where(kk > qq, NEG, 0.0).astype(bf)
    selk = np.zeros((H, 80, 71), np.float32)
    selq = np.zeros((H, 80, 71), np.float32)
    for h in range(H):
        selk[h, h, 67] = -1
        selk[h, 32 + h, 68] = -1
        selk[h, 64 + h, 69] = -1
        selk[h, 16, 64:67] = 1
        selk[h, 17, 70] = 1
        selq[h, h, 64] = 1
        selq[h, 32 + h, 65] = 1
        selq[h, 64 + h, 66] = 1
        selq[h, 16, 67:71] = 1
    bk = t5_band_buckets()
    rel_bias = np.asarray(inputs["rel_bias"], dtype=np.float32)
    idx = np.full((256, 128), -1, np.int64)
    for q in range(128):
        for kl in range(256):
            if q < 64:
                if kl < 192:
                    idx[kl, q] = bk[q, kl]
            else:
                if kl >= 64:
                    idx[kl, q] = bk[q - 64, kl - 64]
    swab = np.full((128, 4, 2, 4, 128), NEG, np.float32)
    valid = idx >= 0
    for g in range(4):
        for hd in range(4):
            h = g * 4 + hd
            full = np.where(valid, rel_bias[np.maximum(idx, 0), h], NEG)
            swab[:, g, 0, hd, :] = full[0:128]
            swab[:, g, 1, hd, :] = full[128:256]
    swab = swab.reshape(128, -1).astype(bf)

    shared = {
        "ident": ident.astype(bf), "identf": ident, "trimask": trimask,
        "selk": selk.astype(bf), "selq": selq.astype(bf), "swab": swab,
        "w_ada": inputs["w_ada"], "b_ada": inputs["b_ada"],
        "g_norm_mix": inputs["g_norm_mix"], "g_norm_ffn": inputs["g_norm_ffn"],
        "g_final": np.asarray(inputs["g_final"]).reshape(1, D),
        "fox_w_in": inputs["fox_w_in"], "fox_b_f": np.asarray(inputs["fox_b_f"]).reshape(H, 1),
        "fox_w_out": inputs["fox_w_out"], "swa_w_in": inputs["swa_w_in"],
        "swa_sinks": inputs["swa_sinks"], "swa_w_out": inputs["swa_w_out"],
        "ffn_w_gu": inputs["ffn_w_gu"], "ffn_w_down": inputs["ffn_w_down"],
        "moe_w_router": inputs["moe_w_router"], "moe_b_router": inputs["moe_b_router"],
        "moe_w_gu": inputs["moe_w_gu"], "moe_w_down": inputs["moe_w_down"],
    }
    if stage < 6:
        del shared["moe_w_gu"], shared["moe_w_down"]
    shared = {k: np.ascontiguousarray(np.asarray(v)) for k, v in shared.items()}
    in_maps = []
    for core in range(8):
        b, half = core // 2, core % 2
        xo = np.zeros((TOWN, D), np.float32)
        xc = np.zeros((TCTX, D), np.float32)
        kb2 = np.zeros((2, S), np.float32)
        kb2[0, :] = 1.0
        halo = np.zeros((128, 1), np.float32)
        if half == 0:
            xo[128:] = x[b, 0:2048]
            kb2[1, 0:TCTX + 128] = NEG
            halo[:] = NEG
        else:
            xo[:] = x[b, TCTX:S]
            xc[:] = x[b, 0:TCTX]
        m = dict(shared)
        m["xo"] = xo
        m["xc"] = xc
        m["cT"] = np.ascontiguousarray(c[b].reshape(8, 128).T)
        m["kb2"] = kb2.astype(bf)
        m["halo"] = halo
        in_maps.append(m)
    return in_maps


_CACHE = {}


def kernel(**inputs):
    stage = int(os.environ.get("KSTAGE", "99"))
    if stage not in _CACHE:
        _CACHE[stage] = build_program(stage)
    nc = _CACHE[stage]
    in_maps = make_inputs(inputs, stage)
    res = run_bass_kernel_spmd(nc, in_maps, core_ids=list(range(8)))
    if stage < 99:
        return res
    out = np.zeros((4, S, D), np.float32)
    for core in range(8):
        b, half = core // 2, core % 2
        out[b, half * 2048:(half + 1) * 2048] = res.results[core]["out"]
    return out
```

```python
import contextlib
import os
import numpy as np
import ml_dtypes
import concourse.bass as bass
import concourse.mybir as mybir
from concourse.bass_utils import run_bass_kernel_spmd

F32 = mybir.dt.float32
BF16 = mybir.dt.bfloat16
AF = mybir.ActivationFunctionType
ALU = mybir.AluOpType
AX = mybir.AxisListType

D = 1024
S = 4096
H = 16
DH = 64
FF = 3584
NE = 8
NOWN = 17
NCTX = 15
TOWN = NOWN * 128
TCTX = NCTX * 128
NEG = -30000.0

ENGS = ['pe', 'act', 'dve', 'pool', 'sp']
SEM_LIMIT = 30000


class Op:
    __slots__ = ('eng', 'fn', 'deps', 'isdma', 'dsem', 'count', 'needed', 'sig', 'key', 'seq')

    def __init__(self, eng, fn):
        self.eng = eng
        self.fn = fn
        self.deps = []
        self.isdma = False
        self.dsem = None
        self.count = 0
        self.needed = False
        self.sig = None
        self.key = None
        self.seq = 0


class DmaSem:
    def __init__(self, prog, name):
        self.sem = prog.new_sem(name)
        self.count = 0
        self.key = ('dma', name, prog.nsem)
        self.hist = []

    def count_before(self, seq):
        c = 0
        for sq, cn in self.hist:
            if sq < seq:
                c = cn
            else:
                break
        return c


_DSIZE = {}


def _dsize(dt):
    if dt not in _DSIZE:
        _DSIZE[dt] = 2 if dt == BF16 else (4 if dt == F32 else int(np.dtype(dt.np).itemsize))
    return _DSIZE[dt]


def _region(ap):
    t = ap.tensor
    sp = t.space
    if sp == 'SB':
        space = 'sb'
        base = t.manual_sbuf_range[0]
    elif sp == 'PSUM':
        space = 'ps_' + t.name
        base = 0
    else:
        return None
    ds = _dsize(t.dtype)
    shape = list(t.shape)
    rowsize = 1
    for d_ in shape[1:]:
        rowsize *= d_
    apl = [(int(a), int(b)) for a, b in ap.ap]
    off = int(ap.offset)
    pcnt = apl[0][1]
    p0 = off // rowsize
    col0 = off % rowsize
    dims = sorted([(st, c) for st, c in apl[1:] if c > 1], reverse=True)
    starts = [col0]
    k = 0
    while k < len(dims) - 1 and len(starts) * dims[k][1] <= 64:
        st, c = dims[k]
        starts = [s0 + i * st for s0 in starts for i in range(c)]
        k += 1
    span = 1
    for st, c in dims[k:]:
        span += (c - 1) * st
    iv = [(base + s0 * ds, base + (s0 + span) * ds) for s0 in starts]
    lo = min(a for a, _ in iv)
    hi = max(b for _, b in iv)
    return (space, p0, p0 + pcnt, lo, hi, iv)


def _overlap(r1, r2):
    if r1[2] <= r2[1] or r2[2] <= r1[1] or r1[4] <= r2[3] or r2[4] <= r1[3]:
        return False
    i1, i2 = r1[5], r2[5]
    if len(i1) == 1 and len(i2) == 1:
        return True
    for a0, a1 in i1:
        for b0, b1 in i2:
            if a0 < b1 and b0 < a1:
                return True
    return False


def _contains(big, small):
    if big[1] > small[1] or big[2] < small[2]:
        return False
    if len(big[5]) == 1:
        return big[3] <= small[3] and big[4] >= small[4]
    if len(big[5]) == len(small[5]):
        return all(a0 <= b0 and a1 >= b1 for (a0, a1), (b0, b1) in zip(big[5], small[5]))
    return False


class Tracker:
    BK = 2048

    def __init__(self):
        self.buckets = {}
        self.rid = 0

    def _bks(self, r):
        return [(r[0], b) for b in range(r[3] // self.BK, (r[4] - 1) // self.BK + 1)]

    def access(self, op, reads, writes):
        deps = {}
        recs = []
        for ap, isw in [(a, False) for a in reads] + [(a, True) for a in writes]:
            if ap is None or isinstance(ap, (int, float)):
                continue
            r = _region(ap)
            if r is None:
                continue
            seen = set()
            for bk in self._bks(r):
                lst = self.buckets.get(bk)
                if not lst:
                    continue
                for rec in lst:
                    if rec[0] in seen:
                        continue
                    seen.add(rec[0])
                    _, rr, rop, risw = rec
                    if not (isw or risw):
                        continue
                    if rop is op:
                        continue
                    if _overlap(r, rr):
                        deps[id(rop)] = rop
            recs.append((r, isw))
        for r, isw in recs:
            self.rid += 1
            rec = (self.rid, r, op, isw)
            for bk in self._bks(r):
                lst = self.buckets.setdefault(bk, [])
                if isw:
                    lst[:] = [x for x in lst if not _contains(r, x[1])]
                else:
                    lst[:] = [x for x in lst if not ((not x[3]) and x[2].key == op.key and _contains(r, x[1]))]
                lst.append(rec)
        return list(deps.values())


class Prog:
    def __init__(self, nc):
        self.nc = nc
        self.es = contextlib.ExitStack()
        self.ops = {e: [] for e in ENGS}
        self.nsem = 0
        self.trk = Tracker()
        self.nseq = 0

    def new_sem(self, name):
        self.nsem += 1
        return self.es.enter_context(self.nc.semaphore(f"{name}_{self.nsem}"))

    def op(self, eng, fn, waits=(), signal=True, reads=(), writes=()):
        o = Op(eng, fn)
        o.key = eng
        self.nseq += 1
        o.seq = self.nseq
        deps = [w for w in waits if w is not None]
        deps += self.trk.access(o, reads, writes)
        o.deps = deps
        self.ops[eng].append(o)
        return o

    def dma(self, eng, dsem, out, in_, waits=()):
        o = Op(eng, lambda e: e.dma_start(out=out, in_=in_))
        o.isdma = True
        o.dsem = dsem
        o.key = dsem.key
        dsem.count += 16
        o.count = dsem.count
        self.nseq += 1
        o.seq = self.nseq
        dsem.hist.append((o.seq, o.count))
        deps = [w for w in waits if w is not None]
        deps += self.trk.access(o, [in_], [out])
        o.deps = deps
        self.ops[eng].append(o)
        return o

    def wait_only(self, eng, waits):
        o = Op(eng, None)
        o.key = eng
        self.nseq += 1
        o.seq = self.nseq
        o.deps = [w for w in waits if w is not None]
        self.ops[eng].append(o)

    def _reduce_deps(self, o):
        best = {}
        for d in o.deps:
            if d.isdma:
                k = d.dsem.key
                if k not in best or d.count > best[k].count:
                    best[k] = d
            else:
                if d.eng == 'pe' and o.eng == 'pe' and not o.isdma:
                    continue
                k = d.eng
                if k not in best or d.seq > best[k].seq:
                    best[k] = d
        o.deps = list(best.values())

    def emit(self):
        nc = self.nc
        for eng in ENGS:
            for o in self.ops[eng]:
                self._reduce_deps(o)
        for eng in ENGS:
            for o in self.ops[eng]:
                for d in o.deps:
                    if d.isdma:
                        continue
                    if d.eng == 'pe' and o.eng == 'pe' and not o.isdma:
                        continue
                    d.needed = True
        for eng in ENGS:
            cnt = SEM_LIMIT
            gen = 0
            sem = None
            for o in self.ops[eng]:
                if o.isdma or not o.needed:
                    continue
                if cnt >= SEM_LIMIT:
                    gen += 1
                    sem = self.new_sem(f"s_{eng}{gen}")
                    cnt = 0
                cnt += 1
                o.sig = (sem, cnt, (eng, gen))
        nwaits = [0]
        with nc.Block() as block:
            def run(eng_name):
                def body(e):
                    waited = {}
                    for o in self.ops[eng_name]:
                        for d in o.deps:
                            if d.isdma:
                                sem, count, key = d.dsem.sem, max(d.count, d.dsem.count_before(o.seq)), d.dsem.key
                            else:
                                if d.sig is None:
                                    continue
                                sem, count, key = d.sig
                            if waited.get(key, 0) >= count:
                                continue
                            waited[key] = count
                            e.wait_ge(sem, count)
                            nwaits[0] += 1
                        if o.fn is None:
                            continue
                        ins = o.fn(e)
                        if o.isdma:
                            ins.then_inc(o.dsem.sem, 16)
                        elif o.sig is not None:
                            ins.then_inc(o.sig[0], 1)
                return body
            block.tensor(run('pe'))
            block.scalar(run('act'))
            block.vector(run('dve'))
            block.gpsimd(run('pool'))
            block.sync(run('sp'))
        self.stats = {e: len(self.ops[e]) for e in ENGS}
        self.stats['waits'] = nwaits[0]

    def close(self):
        self.es.close()


class Ring:
    def __init__(self, bufs):
        self.bufs = bufs
        self.free = [[] for _ in bufs]
        self.i = 0

    def next(self):
        s = self.i % len(self.bufs)
        self.i += 1
        return s, self.bufs[s], list(self.free[s])

    def release(self, slot, evs):
        self.free[slot] = [e for e in evs if e is not None]


A0 = 0
MODB = 69632
CONST = 94208
B0 = 98304
SB_END = 212736
SB_BASE = 16544


def build_program(stage=99):
    nc = bass.Bass("TRN2", target_bir_lowering=False)
    P = Prog(nc)

    def din(name, shape, dt=F32):
        return nc.dram_tensor(name, list(shape), dt, kind="ExternalInput").ap()

    xo_d = din("xo", [TOWN, D])
    xc_d = din("xc", [TCTX, D])
    cT_d = din("cT", [128, 8])
    kb2_d = din("kb2", [2, S], BF16)
    halo_d = din("halo", [128, 1])
    ident_d = din("ident", [128, 128], BF16)
    identf_d = din("identf", [128, 128])
    trim_d = din("trimask", [128, 128], BF16)
    selk_d = din("selk", [H, 80, 71], BF16)
    selq_d = din("selq", [H, 80, 71], BF16)
    swab_d = din("swab", [128, 4 * 2 * 4 * 128], BF16)
    w_ada_d = din("w_ada", [2, D, 6 * D])
    b_ada_d = din("b_ada", [2, 6 * D])
    gmix_d = din("g_norm_mix", [2, D])
    gffn_d = din("g_norm_ffn", [2, D])
    gfin_d = din("g_final", [1, D])
    fwin_d = din("fox_w_in", [1, D, 3 * D + H])
    fbf_d = din("fox_b_f", [H, 1])
    fwout_d = din("fox_w_out", [1, D, D])
    swin_d = din("swa_w_in", [1, D, D + 512])
    ssink_d = din("swa_sinks", [1, H])
    swout_d = din("swa_w_out", [1, D, D])
    wgu_d = din("ffn_w_gu", [1, D, 2 * FF])
    wdn_d = din("ffn_w_down", [1, FF, D])
    wr_d = din("moe_w_router", [1, D, NE])
    br_d = din("moe_b_router", [1, NE])
    if stage >= 6:
        mgu_d = din("moe_w_gu", [1, NE, D, 2 * FF])
        mdn_d = din("moe_w_down", [1, NE, FF, D])
    out_d = nc.dram_tensor("out", [2048, D], F32, kind="ExternalOutput").ap()
    dbg_d = None
    if stage < 99:
        dbg_d = nc.dram_tensor("dbg", [128, NOWN * D], F32, kind="ExternalOutput").ap()
        dbg2_d = nc.dram_tensor("dbg2", [128, 8 * S], BF16, kind="ExternalOutput").ap()
        dbg3_d = nc.dram_tensor("dbg3", [128, S], F32, kind="ExternalOutput").ap()

    ntens = [0]

    def T(name, shape, dt, off):
        ntens[0] += 1
        assert off % 32 == 0, (name, off)
        sz = int(np.prod(shape[1:])) * (2 if dt == BF16 else 4)
        assert off + sz <= SB_END, (name, off, sz)
        return nc.alloc_sbuf_tensor_at(f"{name}{ntens[0]}", list(shape), dt, offset=off + SB_BASE)

    pb = [nc.alloc_psum_tensor(f"pb{i}", [128, 512], F32) for i in range(8)]
    pbb = [p.bitcast(BF16) for p in pb]

    x_res = T("x_res", [128, NOWN, D], F32, A0)
    modt = [T(f"mod{i}", [128, D], F32, MODB + i * 4096) for i in range(6)]
    SH1, GSC1, GT1, SH2, GSC2, GT2 = range(6)
    ident = T("ident", [128, 128], BF16, CONST)
    trim = T("trim", [128, 128], BF16, CONST + 256)
    cond_rep = T("cond_rep", [128, 8, 128], BF16, CONST + 512)
    condT = T("condT", [128, 8], F32, CONST + 2560)
    cond_s = T("cond_s", [128, 8], F32, CONST + 2592)
    ssq = T("ssq", [128, 64], F32, CONST + 2624)
    rstd = T("rstd", [128, 64], F32, CONST + 2880)
    halo_t = T("halo_t", [128, 1], F32, CONST + 3136)
    bf_col = T("bf_col", [H, 1], F32, CONST + 3168)
    esink = T("esink", [128, H], F32, CONST + 3200)
    brb = T("brb", [128, NE], F32, CONST + 3264)
    small = T("small", [128, 64], F32, CONST + 3296)
    identf = T("identf", [128, 128], F32, CONST + 3584)

    ds_c = DmaSem(P, "dc")
    ds_out = DmaSem(P, "dout")

    def _aps(*xs):
        return [x for x in xs if x is not None and not isinstance(x, (int, float))]

    def act(out, in_, func, waits=(), signal=True, **kw):
        return P.op('act', lambda e: e.activation(out=out, in_=in_, func=func, **kw), waits,
                    reads=_aps(in_, kw.get('bias'), kw.get('scale')), writes=_aps(out, kw.get('accum_out')))

    def mm(out, lhsT, rhs, start, stop, waits=(), signal=False):
        return P.op('pe', lambda e: e.matmul(out, lhsT=lhsT, rhs=rhs, start=start, stop=stop), waits,
                    reads=[lhsT, rhs], writes=[out])

    def tr(out, in_, idn, waits=(), signal=False):
        return P.op('pe', lambda e: e.transpose(out=out, in_=in_, identity=idn), waits, reads=[in_, idn], writes=[out])

    def vtt(out, in0, in1, op, waits=(), signal=True, eng='dve'):
        return P.op(eng, lambda e: e.tensor_tensor(out=out, in0=in0, in1=in1, op=op), waits, reads=[in0, in1], writes=[out])

    def vts(out, in0, s1, s2, op0, op1=None, waits=(), signal=True, eng='dve'):
        if op1 is None:
            return P.op(eng, lambda e: e.tensor_scalar(out=out, in0=in0, scalar1=s1, scalar2=None, op0=op0), waits,
                        reads=_aps(in0, s1), writes=[out])
        return P.op(eng, lambda e: e.tensor_scalar(out=out, in0=in0, scalar1=s1, scalar2=s2, op0=op0, op1=op1), waits,
                    reads=_aps(in0, s1, s2), writes=[out])

    def vstt(out, in0, scalar, in1, op0, op1, waits=(), signal=True, eng='dve'):
        return P.op(eng, lambda e: e.scalar_tensor_tensor(out=out, in0=in0, scalar=scalar, in1=in1, op0=op0, op1=op1), waits,
                    reads=_aps(in0, scalar, in1), writes=[out])

    def vcopy(out, in_, waits=(), signal=True, eng='dve'):
        return P.op(eng, lambda e: e.tensor_copy(out=out, in_=in_), waits, reads=[in_], writes=[out])

    def vrecip(out, in_, waits=()):
        return P.op('dve', lambda e: e.reciprocal(out=out, in_=in_), waits, reads=[in_], writes=[out])

    def memset(ap, val, waits=(), signal=True, eng='pool'):
        return P.op(eng, lambda e: e.memset(ap, val), waits, writes=[ap])

    P.dma('sp', ds_c, ident[:], ident_d)
    P.dma('sp', ds_c, identf[:], identf_d)
    P.dma('sp', ds_c, trim[:], trim_d)
    P.dma('sp', ds_c, halo_t[:], halo_d)
    P.dma('sp', ds_c, bf_col[:], fbf_d)
    P.dma('sp', ds_c, esink[:], ssink_d[0:1, :].partition_broadcast(128))
    P.dma('sp', ds_c, brb[:], br_d[0:1, :].partition_broadcast(128))
    e_const = P.dma('sp', ds_c, condT[:], cT_d)
    e_silu = act(cond_s[:], condT[:], AF.Silu, waits=[e_const])
    e_crep = None
    for k in range(8):
        e_crep = act(cond_rep[:, k, :], ident[:], AF.Identity, waits=[e_silu], scale=0.0, bias=cond_s[:, k:k + 1])
    e_esink = act(esink[:], esink[:], AF.Exp)

    def compute_mods(l, base, prior, groups=range(12)):
        wst = [T("wada", [128, 8, 512], BF16, base + i * 8192) for i in range(2)]
        bbs = [T("bb", [128, 512], F32, base + 16384 + i * 2048) for i in range(2)]
        gb = [T("gb", [128, D], F32, base + 20480 + i * 4096) for i in range(2)]
        tmp = T("modtmp", [128, 512], F32, base + 28672)
        dsw = [DmaSem(P, f"dwa{l}{i}") for i in range(2)]
        dsb = [DmaSem(P, f"dbb{l}{i}") for i in range(2)]
        dsg = DmaSem(P, f"dg{l}")
        P.dma('sp', dsg, gb[0][:], gmix_d[l:l + 1, :].partition_broadcast(128), waits=prior)
        e_g = P.dma('sp', dsg, gb[1][:], gffn_d[l:l + 1, :].partition_broadcast(128), waits=prior)
        wring = Ring(wst)
        bring = Ring(bbs)
        pring = Ring([pb[0], pb[1]])
        wv = w_ada_d[l].rearrange("(k p) n -> p k n", p=128)
        last = None
        evs = []
        for cg in groups:
            ws, wt, wfree = wring.next()
            bs, bt, bfree = bring.next()
            ps, pt, pfree = pring.next()
            e_w = P.dma('pool', dsw[ws], wt[:], wv[:, :, cg * 512:(cg + 1) * 512], waits=wfree + list(prior))
            e_b = P.dma('sp', dsb[bs], bt[:], b_ada_d[l:l + 1, cg * 512:(cg + 1) * 512].partition_broadcast(128),
                        waits=bfree + list(prior))
            e_m = None
            for k in range(8):
                e_m = mm(pt[:], cond_rep[:, k, :], wt[:, k, :], k == 0, k == 7,
                         waits=[e_w, e_crep] + pfree, signal=(k == 7))
            idx, half = cg // 2, cg % 2
            dst = modt[idx][:, half * 512:(half + 1) * 512]
            if idx in (1, 4):
                vtt(tmp[:], pt[:], bt[:], ALU.add, waits=[e_m, e_b, last] + list(prior), signal=False)
                g = gb[0] if idx == 1 else gb[1]
                last = vstt(dst, tmp[:], 1.0, g[:, half * 512:(half + 1) * 512], ALU.add, ALU.mult, waits=[e_g])
            else:
                last = vtt(dst, pt[:], bt[:], ALU.add, waits=[e_m, e_b] + list(prior))
            wring.release(ws, [e_m])
            bring.release(bs, [last])
            pring.release(ps, [last])
            evs.append(last)
        return last

    class NormCtx:
        def __init__(self, base, prior):
            self.t = Ring([T("nt", [128, D], F32, base + i * 4096) for i in range(2)])
            self.hb = Ring([T("nhb", [128, D], BF16, base + 8192 + i * 2048) for i in range(2)])
            self.junk = T("njunk", [128, D], BF16, base + 12288)
            self.pt = Ring([pbb[6], pbb[7]])
            self.prior = list(prior)
            self.n = 0
        SIZE = 14336

    def norm_block(ctx, xin, col, gsc_t, sh_t, hT_dst, waits, h32=None):
        pr = ctx.prior if ctx.n < 2 else []
        ctx.n += 1
        e_sq = act(ctx.junk[:], xin, AF.Square, waits=list(waits) + pr, accum_out=ssq[:, col:col + 1])
        e_sd = act(rstd[:, col:col + 1], ssq[:, col:col + 1], AF.Sqrt, waits=[e_sq], scale=1.0 / D, bias=small[:, 0:1])
        e_r = vrecip(rstd[:, col:col + 1], rstd[:, col:col + 1], waits=[e_sd] + pr)
        ts, tt, tfree = ctx.t.next()
        hs, hb, hfree = ctx.hb.next()
        e_t = vstt(tt[:], xin, rstd[:, col:col + 1], gsc_t[:], ALU.mult, ALU.mult, waits=list(waits) + tfree + pr + [e_r])
        if h32 is not None:
            vtt(h32[:], tt[:], sh_t[:], ALU.add)
            e_h = vcopy(hb[:], h32[:], waits=hfree)
        else:
            e_h = vtt(hb[:], tt[:], sh_t[:], ALU.add, waits=hfree)
        ctx.t.release(ts, [e_h])
        ps, pt, pfree = ctx.pt.next()
        e_tr = None
        for k in range(8):
            e_tr = tr(pt[:, k * 128:(k + 1) * 128], hb[:, k * 128:(k + 1) * 128], ident[:],
                      waits=[e_h] + pfree + pr, signal=(k == 7))
        ctx.hb.release(hs, [e_tr])

        def fin():
            e_c = act(hT_dst, pt[:, :].rearrange("p (k t) -> p k t", k=8), AF.Copy, waits=[e_tr])
            ctx.pt.release(ps, [e_c])
            return e_c
        norm_flush(ctx)
        ctx.pending = fin
        return None, e_t, e_sq

    def norm_flush(ctx):
        e = None
        if getattr(ctx, "pending", None) is not None:
            e = ctx.pending()
            ctx.pending = None
        return e

    e_eps = memset(small[:, 0:1], 1e-6, eng='dve')
    e_one = memset(small[:, 1:2], 1.0, eng='dve')

    compute_mods(0, B0, [], range(0, 4))

    hT = T("hT", [128, 8, S], BF16, 0)
    PT = [T("PT", [128, 384], BF16, 65536 + i * 768) for i in range(4)]
    selk_t = [T("selk", [80, 71], BF16, 68608 + i * 160) for i in range(2)]
    selq_t = [T("selq", [80, 71], BF16, 68928 + i * 160) for i in range(2)]
    oT = T("oT", [128, 8, TOWN], BF16, B0)
    o_pair = [T("opair", [128, NOWN, 128], BF16, B0 + 34816 + i * 4352) for i in range(2)]
    V_grp = T("Vgrp", [128, 32, 4, 65], BF16, B0 + 43520)
    Fsrc = T("Fsrc", [80, S], BF16, B0 + 60160)
    KT = [T("KT", [71, S], BF16, B0 + 68352 + i * 8192) for i in range(2)]
    QT = [T("QT", [71, TOWN], BF16, B0 + 84736 + i * 4352) for i in range(2)]
    Wqk = [T("Wqk", [128, 8, 142], BF16, B0 + 93440 + i * 2272) for i in range(2)]
    Wv = T("Wv", [128, 8, 256], BF16, B0 + 97984)
    Wf = T("Wf", [128, 8, 16], BF16, B0 + 102080)
    NB = B0 + 68352
    xin_bufs = [T("xin", [128, D], F32, NB + 14336 + i * 4096) for i in range(2)]

    nctx = NormCtx(NB, [])
    xring = Ring(xin_bufs)
    ds_x = [DmaSem(P, f"dx{i}") for i in range(2)]
    e_hT = None
    for i in range(32):
        src = xc_d[i * 128:(i + 1) * 128, :] if i < NCTX else xo_d[(i - NCTX) * 128:(i - NCTX + 1) * 128, :]
        xs, xb, xfree = xring.next()
        e_ld = P.dma('sp', ds_x[xs], xb[:], src, waits=xfree)
        e_c, e_t, e_sq = norm_block(nctx, xb[:], i, modt[GSC1], modt[SH1], hT[:, :, i * 128:(i + 1) * 128],
                                    [e_ld, e_eps])
        xring.release(xs, [e_t, e_sq])
    e_hT = norm_flush(nctx)
    compute_mods(0, B0, [], range(4, 12))

    if stage == 1:
        e1 = P.dma('sp', ds_out, dbg2_d, hT[:, :, :].rearrange("p k t -> p (k t)"), waits=[e_hT])
        P.wait_only('sp', [e1])
        P.emit()
        P.close()
        return nc


    X1 = T("X1", [H, S], F32, B0)
    X2 = T("X2", [H, 2048], F32, B0 + 16384)
    X3 = T("X3", [H, 2048], F32, B0 + 24576)
    thi = T("thi", [H, 2048], BF16, B0 + 32768)
    tmid = T("tmid", [H, 2048], BF16, B0 + 36864)
    tlo = T("tlo", [H, 2048], BF16, B0 + 40960)
    wvv = fwin_d[0].rearrange("(k p) n -> p k n", p=128)
    ds_wf = DmaSem(P, "dwf")
    e_wf = P.dma('pool', ds_wf, Wf[:], wvv[:, :, 3 * D:3 * D + H])
    e_fz = memset(Fsrc[:], 0.0, eng='pool')
    ds_kb = DmaSem(P, "dkb")
    e_kb = P.dma('sp', ds_kb, Fsrc[16:18, :], kb2_d, waits=[e_fz])
    ds_f = DmaSem(P, "dfs")
    projring = Ring([pb[6], pb[7]])
    projringb = {id(pb[6]): pbb[6], id(pb[7]): pbb[7]}
    e_prev = None
    e_fdma = []
    for c in range(2):
        sl = slice(c * 2048, (c + 1) * 2048)
        for t4 in range(4):
            tg = c * 4 + t4
            ps_, pt, pfree = projring.next()
            e_m = None
            for k in range(8):
                e_m = mm(pt[0:H, :], Wf[:, k, :], hT[:, k, tg * 512:(tg + 1) * 512], k == 0, k == 7,
                         waits=[e_wf, e_hT] + pfree, signal=(k == 7))
            e_z = act(X1[:, tg * 512:(tg + 1) * 512], pt[0:H, :], AF.Identity, waits=[e_m] + e_fdma, bias=bf_col[:, 0:1])
            projring.release(ps_, [e_z])
        e = act(X2[:], X1[:, sl], AF.Abs, waits=[e_z] + e_fdma)
        e = act(X2[:], X2[:], AF.Exp, waits=[e], scale=-1.0)
        e = act(X2[:], X2[:], AF.Ln, waits=[e, e_one], bias=small[0:H, 1:2])
        e = vstt(X3[:], X1[:, sl], 0.0, X2[:], ALU.min, ALU.subtract, waits=[e] + e_fdma)
        e = memset(X2[:], 1.0, waits=[e], eng='dve')
        init = 0.0 if c == 0 else X1[:, c * 2048 - 1:c * 2048]
        e = P.op('dve', lambda en, sl=sl, init=init: en.tensor_tensor_scan(
            out=X1[:, sl], data0=X2[:], data1=X3[:], initial=init, op0=ALU.mult, op1=ALU.add), waits=[e, e_prev],
            reads=_aps(X2[:], X3[:], init), writes=[X1[:, sl]])
        e_prev = e
        e1 = act(thi[:], X1[:, sl], AF.Identity, waits=[e] + e_fdma, scale=8.0)
        e = vstt(X3[:], X1[:, sl], 8.0, thi[:], ALU.mult, ALU.subtract, waits=[e1])
        e2 = act(tmid[:], X3[:], AF.Copy, waits=[e] + e_fdma)
        e = vtt(X2[:], X3[:], tmid[:], ALU.subtract, waits=[e2])
        e3 = act(tlo[:], X2[:], AF.Copy, waits=[e] + e_fdma)
        e_fdma = [P.dma('sp', ds_f, Fsrc[0:16, sl], thi[:], waits=[e1, e_fz]),
                  P.dma('sp', ds_f, Fsrc[32:48, sl], tmid[:], waits=[e2]),
                  P.dma('sp', ds_f, Fsrc[64:80, sl], tlo[:], waits=[e3])]
    e_F = [e_fdma[-1], e_kb]

    if stage == 15:
        e1 = P.dma('sp', ds_out, dbg3_d[0:H, :], X1[:], waits=e_F)
        e2 = P.dma('sp', ds_out, dbg2_d[0:80, 0:S], Fsrc[:], waits=e_F)
        P.wait_only('sp', [e2])
        P.emit()
        P.close()
        return nc

    ds_qk = [DmaSem(P, f"dqk{i}") for i in range(2)]
    ds_sel = [DmaSem(P, f"dsel{i}") for i in range(2)]
    ds_wv = DmaSem(P, "dwv")
    sring = Ring([pb[0], pb[1], pb[5]])
    ptring = Ring(PT)
    Obank = [pb[2], pb[3], pb[4]]
    Ofree = [[], [], []]
    SBS = [(0, 2), (2, 5), (5, 8), (8, 11), (11, 14), (14, 17)]
    QTG = [(0, 512), (512, 512), (1024, 512), (1536, 512), (2048, 128)]

    def head_tiles():
        tl = []
        for (i0, i1) in SBS:
            nq = i1 - i0
            for kb in range(0, NCTX + i1):
                j0 = max(0, kb - NCTX - i0)
                tl.append((i0, nq, kb, j0, kb >= NCTX + i0))
        return tl
    TILES = head_tiles()

    state = {"proj_done": {}, "kq_ready": {}, "wload": {}, "v_ready": None, "v_last_read": [e_fdma[-1]],
             "last_pv": None}

    def load_head_w(h):
        slot = h % 2
        w = state["proj_done"].get(h - 2, [])
        if h < 2:
            memset(Wqk[slot][:, :, 64:71], 0.0, eng='pool')
            memset(Wqk[slot][:, :, 135:142], 0.0, eng='pool')
        P.dma('pool', ds_qk[slot], Wqk[slot][:, :, 0:64], wvv[:, :, h * 64:(h + 1) * 64], waits=w)
        e1 = P.dma('pool', ds_qk[slot], Wqk[slot][:, :, 71:135], wvv[:, :, D + h * 64:D + (h + 1) * 64], waits=w)
        P.dma('sp', ds_sel[slot], selk_t[slot][:], selk_d[h], waits=w)
        e2 = P.dma('sp', ds_sel[slot], selq_t[slot][:], selq_d[h], waits=w)
        state["wload"][h] = [e1, e2]

    def proj_units(h):
        slot = h % 2
        units = []
        evs = []
        state["kq_ready"][h] = evs

        def unit(kind, c0, n, dst):
            def f():
                ps_, pt, pfree = projring.next()
                wl = state["wload"][h] + e_F + pfree
                if kind == 'k':
                    mm(pt[0:71, 0:n], selk_t[slot][:, :], Fsrc[:, c0:c0 + n], True, False, waits=wl)
                    wc = slice(71, 142)
                else:
                    mm(pt[0:71, 0:n], selq_t[slot][:, :], Fsrc[:, c0:c0 + n], True, False, waits=wl)
                    wc = slice(0, 71)
                e_m = None
                for k in range(8):
                    e_m = mm(pt[0:71, 0:n], Wqk[slot][:, k, wc], hT[:, k, c0:c0 + n], False, k == 7, signal=(k == 7))
                e_c = vcopy(dst, pt[0:71, 0:n], waits=[e_m])
                projring.release(ps_, [e_c])
                evs.append(e_c)
                state["proj_done"][h] = [e_m]
            return f
        for tg in range(8):
            units.append(unit('k', tg * 512, 512, KT[slot][:, tg * 512:(tg + 1) * 512]))
        for (st, n) in QTG:
            units.append(unit('q', TCTX + st, n, QT[slot][:, st:st + n]))
        return units

    def compute_V(g):
        w = state["v_last_read"]
        e_w = P.dma('pool', ds_wv, Wv[:], wvv[:, :, 2 * D + g * 256:2 * D + (g + 1) * 256], waits=w)
        evs = []
        if g == 0:
            evs.append(memset(V_grp[:, :, :, 64:65], 1.0, waits=w, eng='pool'))
        for tb2 in range(16):
            ps_, pt, pfree = projring.next()
            e_m = None
            for j in range(2):
                tb = tb2 * 2 + j
                for k in range(8):
                    e_m = mm(pt[:, j * 256:(j + 1) * 256], hT[:, k, tb * 128:(tb + 1) * 128], Wv[:, k, :], k == 0, k == 7,
                             waits=[e_w] + pfree, signal=(j == 1 and k == 7))
            src = pt[:, :].rearrange("p (j h d) -> p j h d", j=2, h=4)
            dst = V_grp[:, tb2 * 2:tb2 * 2 + 2, :, 0:64]
            e_c = vcopy(dst, src, waits=[e_m] + w)
            projring.release(ps_, [e_c])
            evs.append(e_c)
        state["v_ready"] = evs
        state["v_last_read"] = [e_m]

    pend = []

    def emit_S(h, tile):
        i0, nq, kb, j0, diag = tile
        slot = h % 2
        ss, sb_, sfree = sring.next()
        c0, c1 = j0 * 128, nq * 128
        e_s = mm(sb_[:, c0:c1], KT[slot][:, kb * 128:(kb + 1) * 128], QT[slot][:, i0 * 128 + c0:i0 * 128 + c1],
                 True, not diag, waits=state["kq_ready"][h] + sfree, signal=not diag)
        if diag:
            e_s = mm(sb_[:, c0:c0 + 128], ident[:], trim[:], False, True, signal=True)
        return (h, tile, ss, sb_, e_s)

    def emit_EXP_PV(rec):
        h, tile, ss, sb_, e_s = rec
        i0, nq, kb, j0, diag = tile
        c0, c1 = j0 * 128, nq * 128
        ps_, ptb, pfree = ptring.next()
        e_x = act(ptb[:, c0:c1], sb_[:, c0:c1], AF.Exp, waits=[e_s] + pfree, scale=0.125)
        sring.release(ss, [e_x])
        hh = h % 4
        e_pv = None
        for j in range(j0, nq):
            first = (kb == 0)
            last = (kb == NCTX + i0 + j)
            wl = [e_x] + state["v_ready"]
            if first:
                wl = wl + Ofree[j]
            is_last_of_tile = (j == nq - 1)
            e_pv = mm(Obank[j][:, 0:65], ptb[:, j * 128:(j + 1) * 128], V_grp[:, kb, hh, :], first, last,
                      waits=wl, signal=(last or is_last_of_tile))
            if last:
                col = 8 + j
                vts(small[:, col:col + 1], Obank[j][:, 64:65], 1e-30, None, ALU.max, waits=[e_pv])
                e_r = vrecip(small[:, col:col + 1], small[:, col:col + 1])
                pslot = (h // 2) % 2
                e_n = vts(o_pair[pslot][:, i0 + j, hh % 2 * 64:hh % 2 * 64 + 64], Obank[j][:, 0:64],
                          small[:, col:col + 1], None, ALU.mult, waits=[e_r] + state.get("opair_free%d" % pslot, []))
                Ofree[j] = [e_n]
                state["last_norm"] = e_n
        ptring.release(ps_, [e_pv])
        state["last_pv"] = e_pv

    def emit_opair_T(h):
        pair = h // 2
        pslot = pair % 2
        e_last = None
        for i0_ in range(0, NOWN, 8):
            n = min(8, NOWN - i0_)
            ps_, pt, pfree = projring.next()
            ptv = projringb[id(pt)]
            e_t = None
            for i in range(n):
                e_t = tr(ptv[:, i * 128:(i + 1) * 128], o_pair[pslot][:, i0_ + i, :], ident[:],
                         waits=[state["last_norm"]] + pfree, signal=(i == n - 1))
            e_c = vcopy(oT[:, pair, i0_ * 128:(i0_ + n) * 128], ptv[:, 0:n * 128], waits=[e_t])
            projring.release(ps_, [e_c])
            e_last = e_t
        state["opair_free%d" % pslot] = [e_last]
        state["oT_last"] = e_c

    load_head_w(0)
    compute_V(0)
    for u in proj_units(0):
        u()
    for h in range(H):
        nxt = []
        if h + 1 < H:
            load_head_w(h + 1)
            nxt = proj_units(h + 1)
        recs = [emit_S(h, TILES[0]), emit_S(h, TILES[1])]
        nt = len(TILES)
        every = max(1, nt // (len(nxt) + 1)) if nxt else nt + 1
        for t in range(nt):
            if t + 2 < nt:
                recs.append(emit_S(h, TILES[t + 2]))
            emit_EXP_PV(recs[t])
            if nxt and (t % every == every - 1):
                nxt.pop(0)()
        while nxt:
            nxt.pop(0)()
        if h % 2 == 1:
            emit_opair_T(h)
        if h + 1 < H and (h + 1) % 4 == 0:
            state["v_last_read"] = [state["last_pv"]]
            compute_V((h + 1) // 4)

    if stage == 2:
        e1 = P.dma('sp', ds_out, dbg2_d[:, 0:8 * TOWN], oT[:, :, :].rearrange("p k t -> p (k t)"), waits=[state["oT_last"]])
        P.wait_only('sp', [e1])
        P.emit()
        P.close()
        return nc


    def finish_dbg():
        e1 = P.dma('sp', ds_out, dbg_d, x_res[:, :, :].rearrange("p i d -> p (i d)"))
        P.wait_only('sp', [e1])
        P.emit()
        P.close()
        return nc

    def out_proj(w_src, name):
        Wo = T("Wo" + name, [128, 8, D], BF16, B0 + 93440)
        ytmp = [T("ytmp" + name, [128, 512], F32, B0 + 109824 + i * 2048) for i in range(2)]
        ds_wo = DmaSem(P, "dwo" + name)
        wsrc = w_src.rearrange("(k p) n -> p k n", p=128)
        for k2 in range(2):
            P.dma('pool', ds_wo, Wo[:, :, k2 * 512:(k2 + 1) * 512], wsrc[:, :, k2 * 512:(k2 + 1) * 512])
        return Wo, ytmp

    Wo, ytmp = out_proj(fwout_d[0], "f")
    ds_xr = DmaSem(P, "dxr")
    for i in range(NOWN):
        P.dma('sp', ds_xr, x_res[:, i, :], xo_d[i * 128:(i + 1) * 128, :])
    n_y = 0
    for i in range(NOWN):
        for half in range(2):
            bank = pb[n_y % 2]
            yt = ytmp[n_y % 2]
            n_y += 1
            hs = slice(half * 512, (half + 1) * 512)
            for k in range(8):
                mm(bank[:, :], oT[:, k, i * 128:(i + 1) * 128], Wo[:, k, hs], k == 0, k == 7)
            vtt(yt[:], bank[:], modt[GT1][:, hs], ALU.mult)
            vtt(x_res[:, i, hs], x_res[:, i, hs], yt[:], ALU.add)

    if stage == 3:
        return finish_dbg()

    FB = B0
    hT2 = T("hT2", [128, 8, TOWN], BF16, FB)
    actT = [T("actT", [128, 4, 512], BF16, FB + 34816 + i * 4096) for i in range(2)]
    wgu_t = [T("wgu", [128, 8, 1024], BF16, FB + 43008 + i * 16384) for i in range(2)]
    wd_t = [T("wd", [128, 4, D], BF16, FB + 75776 + i * 8192) for i in range(2)]
    sg_t = [T("sg", [128, 512], F32, FB + 92160 + i * 2048) for i in range(2)]
    NB2 = FB + 96256
    gates = T("gates", [128, NOWN * NE], F32, FB + 110592)
    ds_gu = [DmaSem(P, f"dgu{i}") for i in range(2)]
    ds_wd = [DmaSem(P, f"dwd{i}") for i in range(2)]
    ffn_state = {"stage": 0, "g": 0, "u": 0, "y": 0, "a": 0, "s": 0}
    Gb = [pb[0], pb[1]]
    Ub = [pb[2], pb[3]]
    Yb = [pb[4], pb[5]]

    def ffn_pass(gsrc, usrc, dsrc, blk0, blk1, gt_tile, expert=None):
        groups = [(b, min(b + 4, blk1)) for b in range(blk0, blk1, 4)]
        st = ffn_state

        def emit_down(aslot, b0, b1, slot):
            for b in range(b0, b1):
                for half in range(2):
                    Y = Yb[st["y"] % 2]
                    st["y"] += 1
                    hs = slice(half * 512, (half + 1) * 512)
                    for fc in range(4):
                        mm(Y[:, :], actT[aslot][:, fc, (b - b0) * 128:(b - b0 + 1) * 128], wd_t[slot][:, fc, hs], fc == 0, fc == 3)
                    if expert is None:
                        vtt(x_res[:, b, hs], Y[:, :], x_res[:, b, hs], ALU.add)
                    else:
                        vstt(x_res[:, b, hs], Y[:, :], gates[:, b * NE + expert:b * NE + expert + 1], x_res[:, b, hs],
                             ALU.mult, ALU.add)

        for fg in range(7):
            slot = st["stage"] % 2
            st["stage"] += 1
            fs = slice(fg * 512, (fg + 1) * 512)
            P.dma('pool', ds_gu[slot], wgu_t[slot][:, :, 0:512], gsrc[:, :, fs])
            P.dma('pool', ds_gu[slot], wgu_t[slot][:, :, 512:1024], usrc[:, :, fs])
            P.dma('pool', ds_wd[slot], wd_t[slot][:, :, :], dsrc[fg * 512:(fg + 1) * 512, :].rearrange("(c p) n -> p c n", p=128))
            for c in range(4):
                vtt(wd_t[slot][:, c, :], wd_t[slot][:, c, :], gt_tile[:], ALU.mult, eng='pool')
            pending = None
            for (b0, b1) in groups:
                ntok = (b1 - b0) * 128
                t0 = b0 * 128
                aslot = st["a"] % 2
                st["a"] += 1
                for fc in range(4):
                    G = Gb[st["g"] % 2]
                    U = Ub[st["g"] % 2]
                    st["g"] += 1
                    sg = sg_t[st["s"] % 2]
                    st["s"] += 1
                    for k in range(8):
                        mm(G[:, 0:ntok], wgu_t[slot][:, k, fc * 128:(fc + 1) * 128], hT2[:, k, t0:t0 + ntok], k == 0, k == 7)
                    for k in range(8):
                        mm(U[:, 0:ntok], wgu_t[slot][:, k, 512 + fc * 128:512 + (fc + 1) * 128], hT2[:, k, t0:t0 + ntok],
                           k == 0, k == 7)
                    act(sg[:, 0:ntok], G[:, 0:ntok], AF.Silu)
                    vtt(actT[aslot][:, fc, 0:ntok], sg[:, 0:ntok], U[:, 0:ntok], ALU.mult)
                if pending is not None:
                    emit_down(*pending)
                pending = (aslot, b0, b1, slot)
            emit_down(*pending)

    nctx2 = NormCtx(NB2, [])
    for i in range(NOWN):
        norm_block(nctx2, x_res[:, i, :], 32 + i, modt[GSC2], modt[SH2], hT2[:, :, i * 128:(i + 1) * 128], [])
    norm_flush(nctx2)
    gu0 = wgu_d[0].rearrange("(k p) n -> p k n", p=128)
    ffn_pass(gu0[:, :, 0:FF], gu0[:, :, FF:2 * FF], wdn_d[0], 0, NOWN, modt[GT2])

    if stage == 4:
        return finish_dbg()

    e_mod1 = compute_mods(1, FB + 43008, [])

    nctx3 = NormCtx(NB2, [])
    for i in range(NOWN):
        norm_block(nctx3, x_res[:, i, :], i, modt[GSC1], modt[SH1], hT2[:, :, i * 128:(i + 1) * 128], [])
    norm_flush(nctx3)
    KT2 = T("KT2", [128, 4, TOWN], BF16, FB + 34816)
    V2 = T("V2", [128, NOWN, 4, 65], BF16, FB + 52224)
    QT2 = T("QT2", [128, 8, 2048], BF16, FB + 61088)
    swab_t = T("swab", [128, 4 * 2 * 4 * 128], BF16, FB + 93856)
    wst = T("wst", [128, 8, 512], BF16, FB + 93856)
    PT2 = [T("PT2", [128, 512], BF16, FB + 102048 + i * 1024) for i in range(4)]
    otok = [T("otok", [128, D], BF16, FB + 106144 + i * 2048) for i in range(2)]
    oTb = [T("oTb", [128, 8, 128], BF16, FB + 110240 + i * 2048) for i in range(2)]
    Wo2 = T("Wo2", [128, 8, D], BF16, FB)
    ytmp2 = [T("ytmp2", [128, 512], F32, FB + 16384 + i * 2048) for i in range(2)]
    swv = swin_d[0].rearrange("(k p) n -> p k n", p=128)
    ds_ws = DmaSem(P, "dws")
    for g in range(4):
        for dup in range(2):
            P.dma('pool', ds_ws, wst[:, :, g * 128 + dup * 64:g * 128 + dup * 64 + 64], swv[:, :, D + g * 64:D + (g + 1) * 64])
    npj = 0
    for (st_, n) in [(0, 512), (512, 512), (1024, 512), (1536, 512), (2048, 128)]:
        for g in range(4):
            pt = pb[6 + npj % 2]
            npj += 1
            for k in range(8):
                mm(pt[:, 0:n], wst[:, k, g * 128:(g + 1) * 128], hT2[:, k, st_:st_ + n], k == 0, k == 7)
            if npj % 2 == 0:
                act(KT2[:, g, st_:st_ + n], pt[:, 0:n], AF.Copy)
            else:
                vcopy(KT2[:, g, st_:st_ + n], pt[:, 0:n])
    memset(V2[:, :, :, 64:65], 1.0, eng='pool')
    P.dma('pool', ds_ws, wst[:, :, 0:256], swv[:, :, D + 256:D + 512])
    for i in range(NOWN):
        pt = pb[6 + npj % 2]
        npj += 1
        for k in range(8):
            mm(pt[:, 0:256], hT2[:, k, i * 128:(i + 1) * 128], wst[:, k, 0:256], k == 0, k == 7)
        src = pt[:, 0:256].rearrange("p (g d) -> p g d", g=4)
        if npj % 2 == 0:
            act(V2[:, i, :, 0:64], src, AF.Copy)
        else:
            vcopy(V2[:, i, :, 0:64], src)
    for qh in range(2):
        P.dma('pool', ds_ws, wst[:, :, :], swv[:, :, qh * 512:(qh + 1) * 512])
        for pr_ in range(4):
            pair = qh * 4 + pr_
            for tg in range(4):
                pt = pb[6 + npj % 2]
                npj += 1
                for k in range(8):
                    mm(pt[:, :], wst[:, k, pr_ * 128:(pr_ + 1) * 128], hT2[:, k, 128 + tg * 512:128 + (tg + 1) * 512], k == 0, k == 7)
                act(QT2[:, pair, tg * 512:(tg + 1) * 512], pt[:, :], AF.Identity, scale=0.125)
    ds_w2 = DmaSem(P, "dw2")
    wo2src = swout_d[0].rearrange("(k p) n -> p k n", p=128)
    for k2 in range(2):
        P.dma('pool', ds_w2, Wo2[:, :, k2 * 512:(k2 + 1) * 512], wo2src[:, :, k2 * 512:(k2 + 1) * 512])
    ds_swab = DmaSem(P, "dswab")
    P.dma('sp', ds_swab, swab_t[:], swab_d)
    n_pt = 0
    n_y = 0
    swa_ofree = [[], []]
    for i in range(1, NOWN):
        ot = otok[i % 2]
        for g in range(4):
            pts = []
            npair = (i * 4 + g) % 2
            for p_ in range(2):
                Sb = pb[2 * npair + p_]
                kblk = i - 1 + p_
                for hd in range(4):
                    h = 4 * g + hd
                    pair, par = h // 2, h % 2
                    b0_ = (g * 2 + p_) * 512 + hd * 128
                    mm(Sb[:, hd * 128:(hd + 1) * 128], ident[:], swab_t[:, b0_:b0_ + 128], True, False)
                    mm(Sb[:, hd * 128:(hd + 1) * 128], KT2[par * 64:(par + 1) * 64, g, kblk * 128:(kblk + 1) * 128],
                       QT2[par * 64:(par + 1) * 64, pair, (i - 1) * 128:i * 128], False, True)
                ptb = PT2[n_pt % 4]
                n_pt += 1
                if i == 1 and p_ == 0:
                    act(ptb[:, :], Sb[:, :], AF.Exp, bias=halo_t[:, 0:1])
                else:
                    act(ptb[:, :], Sb[:, :], AF.Exp)
                pts.append(ptb)
            Ob = pb[4 + npair]
            e_pv = None
            for hd in range(4):
                oc = hd * 128
                for p_ in range(2):
                    e_pv = mm(Ob[:, oc:oc + 65], pts[p_][:, hd * 128:(hd + 1) * 128], V2[:, i - 1 + p_, g, :], p_ == 0, p_ == 1,
                              waits=swa_ofree[npair])
            Obv = Ob[:, :].rearrange("p (h c) -> p h c", c=128)
            c0_ = 16 + 4 * npair
            den = small[:, c0_:c0_ + 4]
            vtt(den, Obv[:, :, 64], esink[:, 4 * g:4 * g + 4], ALU.add, waits=[e_pv])
            vrecip(den, den)
            denb = den.rearrange("p (a o) -> p a o", o=1).broadcast_to([128, 4, 64])
            e_n = vtt(ot[:, g * 256:(g + 1) * 256].rearrange("p (h d) -> p h d", h=4), Obv[:, :, 0:64], denb, ALU.mult)
            swa_ofree[npair] = [e_n]
        ob = oTb[i % 2]
        ptv = pbb[6]
        for k in range(8):
            tr(ptv[:, k * 128:(k + 1) * 128], ot[:, k * 128:(k + 1) * 128], ident[:])
        act(ob[:, :, :], ptv[:, :].rearrange("p (k t) -> p k t", k=8), AF.Copy)
        for half in range(2):
            yt = ytmp2[n_y % 2]
            n_y += 1
            hs = slice(half * 512, (half + 1) * 512)
            for k in range(8):
                mm(pb[7][:, :], ob[:, k, :], Wo2[:, k, hs], k == 0, k == 7)
            vtt(yt[:], pb[7][:, :], modt[GT1][:, hs], ALU.mult)
            vtt(x_res[:, i, hs], x_res[:, i, hs], yt[:], ALU.add)

    if stage == 5:
        return finish_dbg()

    h32s = [T("h32", [128, D], F32, FB + 43008 + i * 8448) for i in range(2)]
    hT32s = [T("hT32", [128, 8, 128], F32, FB + 43008 + 4096 + i * 8448) for i in range(2)]
    wr32 = T("wr32", [128, 8, NE], F32, FB + 43008 + 16896)
    RB = FB + 43008 + 17152
    LA = T("LA", [128, 16, NE], F32, RB)
    EQ1 = T("EQ1", [128, 16, NE], F32, RB + 512)
    L2 = T("L2", [128, 16, NE], F32, RB + 1024)
    EQ2 = T("EQ2", [128, 16, NE], F32, RB + 1536)
    M1 = T("M1", [128, 16], F32, RB + 2048)
    M2 = T("M2", [128, 16], F32, RB + 2112)
    Dm = T("Dm", [128, 16], F32, RB + 2176)
    Ed = T("Ed", [128, 16], F32, RB + 2240)
    W1 = T("W1", [128, 16], F32, RB + 2304)
    W2 = T("W2", [128, 16], F32, RB + 2368)
    ds_wr = DmaSem(P, "dwr")
    P.dma('sp', ds_wr, wr32[:], wr_d[0].rearrange("(k p) n -> p k n", p=128))
    nctx4 = NormCtx(NB2, [])
    for i in range(1, NOWN):
        h32, hT32 = h32s[i % 2], hT32s[i % 2]
        norm_block(nctx4, x_res[:, i, :], 32 + i, modt[GSC2], modt[SH2], hT2[:, :, i * 128:(i + 1) * 128], [], h32=h32)
        for half in range(2):
            pt = pb[half + 2 * (i % 2)]
            for k4 in range(4):
                k = half * 4 + k4
                tr(pt[:, k4 * 128:(k4 + 1) * 128], h32[:, k * 128:(k + 1) * 128], identf[:])
            vcopy(hT32[:, half * 4:half * 4 + 4, :], pt[:, :].rearrange("p (k t) -> p k t", k=4))
        lgb = pb[4 + i % 2]
        for k in range(8):
            mm(lgb[:, 0:NE], hT32[:, k, :], wr32[:, k, :], k == 0, k == 7)
        vtt(LA[:, i - 1, :], lgb[:, 0:NE], brb[:], ALU.add)
    norm_flush(nctx4)

    def bc(v):
        return v.rearrange("p (a o) -> p a o", o=1).broadcast_to([128, 16, NE])
    P.op('dve', lambda e: e.tensor_reduce(out=M1[:], in_=LA[:], axis=AX.X, op=ALU.max), reads=[LA[:]], writes=[M1[:]])
    vtt(EQ1[:], LA[:], bc(M1[:]), ALU.is_equal)
    vstt(L2[:], EQ1[:], -1e30, LA[:], ALU.mult, ALU.add)
    P.op('dve', lambda e: e.tensor_reduce(out=M2[:], in_=L2[:], axis=AX.X, op=ALU.max), reads=[L2[:]], writes=[M2[:]])
    vtt(EQ2[:], L2[:], bc(M2[:]), ALU.is_equal)
    vtt(Dm[:], M2[:], M1[:], ALU.subtract)
    act(Ed[:], Dm[:], AF.Exp)
    vts(W1[:], Ed[:], 1.0, None, ALU.add)
    vrecip(W1[:], W1[:])
    vtt(W2[:], Ed[:], W1[:], ALU.mult)
    vtt(EQ1[:], EQ1[:], bc(W1[:]), ALU.mult)
    vtt(EQ2[:], EQ2[:], bc(W2[:]), ALU.mult)
    vtt(gates[:, NE:NOWN * NE].rearrange("p (a b) -> p a b", b=NE), EQ1[:], EQ2[:], ALU.add)

    for e_ in range(NE):
        mg = mgu_d[0, e_].rearrange("(k p) n -> p k n", p=128)
        ffn_pass(mg[:, :, 0:FF], mg[:, :, FF:2 * FF], mdn_d[0, e_], 1, NOWN, modt[GT2], expert=e_)

    if stage == 6:
        return finish_dbg()

    gfin = modt[SH1]
    ds_gf = DmaSem(P, "dgf")
    P.dma('sp', ds_gf, gfin[:], gfin_d[0:1, :].partition_broadcast(128))
    fo = [T("fo", [128, D], F32, NB2 + i * 4096) for i in range(2)]
    fjunk = T("fjunk", [128, D], BF16, NB2 + 8192)
    last = []
    for i in range(1, NOWN):
        col = i
        act(fjunk[:], x_res[:, i, :], AF.Square, accum_out=ssq[:, col:col + 1])
        act(rstd[:, col:col + 1], ssq[:, col:col + 1], AF.Sqrt, scale=1.0 / D, bias=small[:, 0:1])
        vrecip(rstd[:, col:col + 1], rstd[:, col:col + 1])
        f = fo[i % 2]
        vstt(f[:], x_res[:, i, :], rstd[:, col:col + 1], gfin[:], ALU.mult, ALU.mult)
        last.append(P.dma('sp', ds_out, out_d[(i - 1) * 128:i * 128, :], f[:]))
    P.wait_only('sp', last)
    P.emit()
    P.close()
    return nc


def t5_band_buckets():
    CHUNK, BAND, WC = 64, 192, 2
    q = np.arange(CHUNK)[:, None]
    k = np.arange(BAND)[None, :] - WC * CHUNK
    rel = k - q
    nb = 16
    max_exact = nb // 2
    ret = (rel > 0).astype(np.int32) * nb
    n = np.abs(rel)
    large = max_exact + (np.log(np.maximum(n, 1) / max_exact)
                         / np.log(128 / max_exact) * (nb - max_exact)).astype(np.int32)
    large = np.minimum(large, nb - 1)
    return (ret + np.where(n < max_exact, n, large)).astype(np.int32)


def make_inputs(inputs, stage=99):
    bf = ml_dtypes.bfloat16
    x = np.asarray(inputs["x"], dtype=np.float32)
    c = np.asarray(inputs["c"], dtype=np.float32)
    ident = np.eye(128, dtype=np.float32)
    kk = np.arange(128)[:, None]
    qq = np.arange(128)[None, :]
    trimask = np.where(kk > qq, NEG, 0.0).astype(bf)
    selk = np.zeros((H, 80, 71), np.float32)
    selq = np.zeros((H, 80, 71), np.float32)
    for h in range(H):
        selk[h, h, 67] = -1
        selk[h, 32 + h, 68] = -1
        selk[h, 64 + h, 69] = -1
        selk[h, 16, 64:67] = 1
        selk[h, 17, 70] = 1
        selq[h, h, 64] = 1
        selq[h, 32 + h, 65] = 1
        selq[h, 64 + h, 66] = 1
        selq[h, 16, 67:71] = 1
    bk = t5_band_buckets()
    rel_bias = np.asarray(inputs["rel_bias"], dtype=np.float32)
    idx = np.full((256, 128), -1, np.int64)
    for q in range(128):
        for kl in range(256):
            if q < 64:
                if kl < 192:
                    idx[kl, q] = bk[q, kl]
            else:
                if kl >= 64:
                    idx[kl, q] = bk[q - 64, kl - 64]
    swab = np.full((128, 4, 2, 4, 128), NEG, np.float32)
    valid = idx >= 0
    for g in range(4):
        for hd in range(4):
            h = g * 4 + hd
            full = np.where(valid, rel_bias[np.maximum(idx, 0), h], NEG)
            swab[:, g, 0, hd, :] = full[0:128]
            swab[:, g, 1, hd, :] = full[128:256]
    swab = swab.reshape(128, -1).astype(bf)

    shared = {
        "ident": ident.astype(bf), "identf": ident, "trimask": trimask,
        "selk": selk.astype(bf), "selq": selq.astype(bf), "swab": swab,
        "w_ada": inputs["w_ada"], "b_ada": inputs["b_ada"],
        "g_norm_mix": inputs["g_norm_mix"], "g_norm_ffn": inputs["g_norm_ffn"],
        "g_final": np.asarray(inputs["g_final"]).reshape(1, D),
        "fox_w_in": inputs["fox_w_in"], "fox_b_f": np.asarray(inputs["fox_b_f"]).reshape(H, 1),
        "fox_w_out": inputs["fox_w_out"], "swa_w_in": inputs["swa_w_in"],
        "swa_sinks": inputs["swa_sinks"], "swa_w_out": inputs["swa_w_out"],
        "ffn_w_gu": inputs["ffn_w_gu"], "ffn_w_down": inputs["ffn_w_down"],
        "moe_w_router": inputs["moe_w_router"], "moe_b_router": inputs["moe_b_router"],
        "moe_w_gu": inputs["moe_w_gu"], "moe_w_down": inputs["moe_w_down"],
    }
    if stage < 6:
        del shared["moe_w_gu"], shared["moe_w_down"]
    shared = {k: np.ascontiguousarray(np.asarray(v)) for k, v in shared.items()}
    in_maps = []
    for core in range(8):
        b, half = core // 2, core % 2
        xo = np.zeros((TOWN, D), np.float32)
        xc = np.zeros((TCTX, D), np.float32)
        kb2 = np.zeros((2, S), np.float32)
        kb2[0, :] = 1.0
        halo = np.zeros((128, 1), np.float32)
        if half == 0:
            xo[128:] = x[b, 0:2048]
            kb2[1, 0:TCTX + 128] = NEG
            halo[:] = NEG
        else:
            xo[:] = x[b, TCTX:S]
            xc[:] = x[b, 0:TCTX]
        m = dict(shared)
        m["xo"] = xo
        m["xc"] = xc
        m["cT"] = np.ascontiguousarray(c[b].reshape(8, 128).T)
        m["kb2"] = kb2.astype(bf)
        m["halo"] = halo
        in_maps.append(m)
    return in_maps


_CACHE = {}


def kernel(**inputs):
    stage = int(os.environ.get("KSTAGE", "99"))
    if stage not in _CACHE:
        _CACHE[stage] = build_program(stage)
    nc = _CACHE[stage]
    in_maps = make_inputs(inputs, stage)
    res = run_bass_kernel_spmd(nc, in_maps, core_ids=list(range(8)))
    if stage < 99:
        return res
    out = np.zeros((4, S, D), np.float32)
    for core in range(8):
        b, half = core // 2, core % 2
        out[b, half * 2048:(half + 1) * 2048] = res.results[core]["out"]
    return out
```
